# Optimizing a Trainium2 kernel written in Bass

```python
import jax, jax.numpy as jnp
from jax import lax
import numpy as np

D_MODEL = 1024
BATCH = 8
SEQ = 4096
DEPTH = 1

CHUNK = 64
HG_HEADS = 8
HG_DK = 64
HG_DV = 64
HG_WIDTH = HG_HEADS * HG_DK
CONV_WIDTH = 512
CONV_K = 3
N_BRANCH = 2
PEER_HEADS = 8
PEER_NKEYS = 128
PEER_N = PEER_NKEYS * PEER_NKEYS
PEER_QDIM = 256
PEER_HALF = PEER_QDIM // 2
PEER_TOPK = 16
PEER_BLOCK = 128
EPS = 1e-6
IN_SPLITS = [HG_WIDTH] * 4 + [CONV_WIDTH] * 3 + [N_BRANCH * D_MODEL]
IN_COLS = sum(IN_SPLITS)

kernel_name = "hybrid_hgrn2_shortconv_peer_block"


def rmsnorm(x, g):
    xf = x.astype(jnp.float32)
    y = xf * lax.rsqrt(jnp.mean(xf * xf, axis=-1, keepdims=True) + EPS)
    return (y * g.astype(jnp.float32)).astype(x.dtype)


def hgrn2_chunkwise(q, k, log_f, v):
    b_, s_, h_, dk = q.shape
    dv = v.shape[-1]
    nc = s_ // CHUNK

    def to_chunks(t):
        return t.astype(jnp.float32).reshape(b_, nc, CHUNK, h_, t.shape[-1]).transpose(1, 0, 3, 2, 4)

    qc, kc, fc, vc = to_chunks(q), to_chunks(k), to_chunks(log_f), to_chunks(v)
    causal = jnp.tril(jnp.ones((CHUNK, CHUNK), dtype=bool))

    def step(state, inp):
        q_, k_, lf, v_ = inp
        b = jnp.cumsum(lf, axis=-2)
        b_last = b[..., -1:, :]
        o_inter = jnp.einsum('bhtd,bhde->bhte', q_ * jnp.exp(b), state)
        rel = jnp.where(causal[:, :, None], b[..., :, None, :] - b[..., None, :, :], -jnp.inf)
        scores = jnp.einsum('bhtd,bhsd,bhtsd->bhts', q_, k_, jnp.exp(rel))
        o = o_inter + jnp.einsum('bhts,bhse->bhte', scores, v_)
        new_state = (jnp.exp(b_last[..., 0, :])[..., None] * state
                     + jnp.einsum('bhsd,bhse->bhde', k_ * jnp.exp(b_last - b), v_))
        return new_state, o

    s0 = jnp.zeros((b_, h_, dk, dv), jnp.float32)
    _, o = lax.scan(step, s0, (qc, kc, fc, vc))
    return o.transpose(1, 0, 3, 2, 4).reshape(b_, s_, h_, dv)


def causal_depthwise_conv(u, w):
    c = u.shape[-1]
    return lax.conv_general_dilated(
        u, w[:, None, :].astype(u.dtype), window_strides=(1,), padding=[(CONV_K - 1, 0)],
        dimension_numbers=('NWC', 'WIO', 'NWC'), feature_group_count=c)


def peer_ffn(h, w_query, keys1, keys2, expert_u, expert_v):
    b_, s_, d = h.shape
    t = b_ * s_
    hf = h.reshape(t, d)
    q = (hf @ w_query).reshape(t, PEER_HEADS, 2, PEER_HALF)
    s1 = jnp.einsum('thc,hnc->thn', q[:, :, 0], keys1)
    s2 = jnp.einsum('thc,hnc->thn', q[:, :, 1], keys2)
    v1, i1 = lax.top_k(s1, PEER_TOPK)
    v2, i2 = lax.top_k(s2, PEER_TOPK)
    cand_s = (v1[..., :, None] + v2[..., None, :]).reshape(t, PEER_HEADS, PEER_TOPK * PEER_TOPK)
    cand_i = (i1[..., :, None] * PEER_NKEYS + i2[..., None, :]).reshape(t, PEER_HEADS, PEER_TOPK * PEER_TOPK)
    top_s, top_pos = lax.top_k(cand_s, PEER_TOPK)
    idx = jnp.take_along_axis(cand_i, top_pos, axis=-1)
    gate = jax.nn.softmax(top_s.astype(jnp.float32), axis=-1).astype(h.dtype)
    nb = t // PEER_BLOCK

    def block(args):
        hb, ib, gb = args
        act = jax.nn.gelu(jnp.einsum('thkd,td->thk', expert_u[ib], hb), approximate=False)
        return jnp.einsum('thk,thkd->td', gb * act, expert_v[ib])

    out = lax.map(block, (hf.reshape(nb, PEER_BLOCK, d),
                          idx.reshape(nb, PEER_BLOCK, PEER_HEADS, PEER_TOPK),
                          gate.reshape(nb, PEER_BLOCK, PEER_HEADS, PEER_TOPK)))
    return out.reshape(b_, s_, d)


def setup_inputs(seed: int = 0) -> dict:
    key = jax.random.key(seed)
    ks = jax.random.split(key, 18)
    f32 = jnp.float32
    nrm = lambda k, shape, scale: jax.random.normal(k, shape, f32) * scale
    gain = lambda k, shape: 1.0 + 0.02 * jax.random.normal(k, shape, f32)
    return {
        "x": jax.random.normal(ks[0], (BATCH, SEQ, D_MODEL), f32),
        "norm_mix_g": gain(ks[1], (DEPTH, D_MODEL)),
        "w_in": nrm(ks[2], (DEPTH, D_MODEL, IN_COLS), D_MODEL ** -0.5),
        "hg_lb_logits": nrm(ks[3], (DEPTH + 1, HG_WIDTH), 0.1),
        "hg_out_norm_g": gain(ks[4], (DEPTH, HG_WIDTH)),
        "conv_w": nrm(ks[5], (DEPTH, CONV_K, CONV_WIDTH), CONV_K ** -0.5),
        "w_branch_hg": nrm(ks[6], (DEPTH, HG_WIDTH, D_MODEL), HG_WIDTH ** -0.5),
        "w_branch_conv": nrm(ks[7], (DEPTH, CONV_WIDTH, D_MODEL), CONV_WIDTH ** -0.5),
        "w_out": nrm(ks[8], (DEPTH, D_MODEL, D_MODEL), D_MODEL ** -0.5),
        "norm_ffn_g": gain(ks[9], (DEPTH, D_MODEL)),
        "peer_w_query": nrm(ks[10], (DEPTH, D_MODEL, PEER_HEADS * PEER_QDIM), D_MODEL ** -0.5),
        "peer_keys1": nrm(ks[11], (DEPTH, PEER_HEADS, PEER_NKEYS, PEER_HALF), PEER_HALF ** -0.5),
        "peer_keys2": nrm(ks[12], (DEPTH, PEER_HEADS, PEER_NKEYS, PEER_HALF), PEER_HALF ** -0.5),
        "peer_u": nrm(ks[13], (DEPTH, PEER_N, D_MODEL), D_MODEL ** -0.5),
        "peer_v": nrm(ks[14], (DEPTH, PEER_N, D_MODEL), PEER_HEADS ** -0.5),
        "norm_final_g": gain(ks[15], (D_MODEL,)),
    }


def reference(x, norm_mix_g, w_in, hg_lb_logits, hg_out_norm_g, conv_w, w_branch_hg,
              w_branch_conv, w_out, norm_ffn_g, peer_w_query, peer_keys1, peer_keys2,
              peer_u, peer_v, norm_final_g):
    b_, s_, _ = x.shape
    lb_all = jnp.cumsum(jax.nn.softmax(hg_lb_logits.astype(jnp.float32), axis=0), axis=0)
    split_at = list(np.cumsum(IN_SPLITS)[:-1])
    for l in range(DEPTH):
        h = rmsnorm(x, norm_mix_g[l])
        proj = h @ w_in[l]
        hq, hf, hi, hg, cb, cc, ch, gates = jnp.split(proj, split_at, axis=-1)

        lb = lb_all[l]
        q = jax.nn.silu(hq.astype(jnp.float32)) * (HG_DK ** -0.5)
        sig = jax.nn.sigmoid(hf.astype(jnp.float32))
        forget = lb + (1.0 - lb) * sig
        k = 1.0 - forget
        log_f = jnp.log(forget)
        rs = lambda t: t.reshape(b_, s_, HG_HEADS, -1)
        o = hgrn2_chunkwise(rs(q), rs(k), rs(log_f), rs(hi))
        o = o * lax.rsqrt(jnp.mean(o * o, axis=-1, keepdims=True) + EPS)
        o = o * hg_out_norm_g[l].astype(jnp.float32).reshape(HG_HEADS, HG_DV)
        y_a = (o.reshape(b_, s_, HG_WIDTH) * jax.nn.silu(hg.astype(jnp.float32))).astype(x.dtype)

        y_b = cb * causal_depthwise_conv(cc * ch, conv_w[l])

        g_a, g_b = jnp.split(jax.nn.sigmoid(gates), 2, axis=-1)
        merged = g_a * (y_a @ w_branch_hg[l]) + g_b * (y_b @ w_branch_conv[l])
        x = x + merged @ w_out[l]

        x = x + peer_ffn(rmsnorm(x, norm_ffn_g[l]), peer_w_query[l], peer_keys1[l],
                         peer_keys2[l], peer_u[l], peer_v[l])
    return rmsnorm(x, norm_final_g)
```

```python
import math
from contextlib import ExitStack
import numpy as np
import concourse.bass as bass
import concourse.mybir as mybir
from concourse.bass_utils import run_bass_kernel_spmd

F32 = mybir.dt.float32
BF16 = mybir.dt.bfloat16
U32 = mybir.dt.uint32
AF = mybir.ActivationFunctionType
ALU = mybir.AluOpType
AX = mybir.AxisListType

D = 1024
INC = 5632
EPS = 1e-6
NEG = -1.0e30


class Res:
    __slots__ = ("name", "w", "readers", "dreads", "dsem", "dcount")

    def __init__(self, name):
        self.name = name
        self.w = None
        self.readers = {}
        self.dreads = []
        self.dsem = None
        self.dcount = 0


class Eng:
    def __init__(self, name, obj, sem):
        self.name = name
        self.obj = obj
        self.sem = sem
        self.count = 0
        self.waited = {}


class Sched:
    def __init__(self, nc, es):
        self.nc = nc
        self.es = es
        self.engs = {}
        for name, obj in (("pe", nc.tensor), ("act", nc.scalar), ("dve", nc.vector),
                          ("pool", nc.gpsimd), ("sp", nc.sync)):
            sem = es.enter_context(nc.semaphore("sem_" + name))
            self.engs[name] = Eng(name, obj, sem)
        self.dsems = []
        self.nres = 0

    def res(self, name=None):
        self.nres += 1
        return Res(name or f"r{self.nres}")

    def _wait(self, eng, sem, val):
        key = id(sem)
        if eng.waited.get(key, 0) < val:
            eng.obj.wait_ge(sem, val)
            eng.waited[key] = val

    def _deps(self, engname, reads, writes, multipart=False):
        deps = []
        for r in reads:
            if r.w is not None:
                deps.append(r.w)
        for w in writes:
            if w.w is not None:
                same_pe = (w.w[0] == 'e' and w.w[1] == engname == 'pe')
                same_multipart = (multipart and w.w[0] == 'd' and w.dsem is not None and w.w[1] is w.dsem)
                if not (same_pe or same_multipart):
                    deps.append(w.w)
            for en, idx in w.readers.items():
                deps.append(('e', en, idx))
            for sem, val in w.dreads:
                deps.append(('d', sem, val))
        return deps

    def _emit_waits(self, eng, deps):
        for d in deps:
            if d[0] == 'e':
                self._wait(eng, self.engs[d[1]].sem, d[2])
            else:
                self._wait(eng, d[1], d[2])

    def op(self, engname, fn, reads=(), writes=()):
        eng = self.engs[engname]
        self._emit_waits(eng, self._deps(engname, reads, writes))
        inst = fn(eng.obj)
        eng.count += 1
        inst.then_inc(eng.sem, 1)
        for r in reads:
            r.readers[engname] = eng.count
        for w in writes:
            w.w = ('e', engname, eng.count)
            w.readers = {}
            w.dreads = []
        return inst

    def dma(self, engname, out, in_, reads=(), writes=(), semres=None, multipart=False, **kw):
        eng = self.engs[engname]
        self._emit_waits(eng, self._deps(engname, reads, writes, multipart))
        sr = semres if semres is not None else (writes[0] if writes else reads[0])
        if sr.dsem is None:
            sr.dsem = self.es.enter_context(self.nc.semaphore("dsem_%s_%d" % (sr.name, len(self.dsems))))
            self.dsems.append(sr)
        sr.dcount += 16
        inst = eng.obj.dma_start(out=out, in_=in_, **kw)
        inst.then_inc(sr.dsem, 16)
        for r in reads:
            r.dreads.append((sr.dsem, sr.dcount))
        for w in writes:
            w.w = ('d', sr.dsem, sr.dcount)
            w.readers = {}
            w.dreads = []
        return inst

    def sync_self(self, engname):
        eng = self.engs[engname]
        if eng.count > 0:
            self._wait(eng, eng.sem, eng.count)

    def barrier(self):
        for e in self.engs.values():
            for f in self.engs.values():
                if f is not e and f.count > 0:
                    self._wait(e, f.sem, f.count)
            for sr in self.dsems:
                if sr.dcount > 0:
                    self._wait(e, sr.dsem, sr.dcount)


class Prog:
    def __init__(self, T, dbg=None):
        self.T = T
        self.dbg = dbg
        nc = bass.Bass("TRN2", target_bir_lowering=False)
        self.nc = nc
        dt = nc.dram_tensor
        self.x = dt("x", [T, D], F32, kind="ExternalInput").ap()
        self.w_in = dt("w_in", [D, INC], F32, kind="ExternalInput").ap()
        self.pa = dt("pa", [512, D], F32, kind="ExternalInput").ap()
        self.pb = dt("pb", [512, D], F32, kind="ExternalInput").ap()
        self.wo = dt("wo", [D, D], F32, kind="ExternalInput").ap()
        self.wq = dt("wq", [D, 2048], F32, kind="ExternalInput").ap()
        self.keysT = dt("keysT", [128, 16 * 128], F32, kind="ExternalInput").ap()
        self.uT = dt("uT", [128 * 128, 1024], F32, kind="ExternalInput").ap()
        self.vC = dt("vC", [128 * 128, 1024], F32, kind="ExternalInput").ap()
        self.g1bc = dt("g1bc", [128, D], F32, kind="ExternalInput").ap()
        self.g2bc = dt("g2bc", [128, D], F32, kind="ExternalInput").ap()
        self.gfbc = dt("gfbc", [128, D], F32, kind="ExternalInput").ap()
        self.gobc = dt("gobc", [64, 512], F32, kind="ExternalInput").ap()
        self.lbl = dt("lbl", [64, 16], F32, kind="ExternalInput").ap()
        self.convw = dt("convw", [128, 12], F32, kind="ExternalInput").ap()
        self.y = dt("y", [T, D], F32, kind="ExternalOutput").ap()
        self.uvb = dt("uvb", [128 * 128, 2048], BF16, kind="Internal").ap()
        self.x2s = dt("x2s", [T, D], F32, kind="Internal").ap()
        self.idxw_d = dt("idxw_d", [128, 3, T], BF16, kind="Internal").ap()
        if dbg:
            self.dbgt = dt("dbg", list(dbg), F32, kind="ExternalOutput").ap()
        with ExitStack() as es:
            self.es = es
            self.S = Sched(nc, es)
            self.build()

    def sb(self, es, name, shape, dtype):
        return es.enter_context(self.nc.sbuf_tensor(name, list(shape), dtype))

    def build(self):
        nc, S, es = self.nc, self.S, self.es
        T = self.T
        self.ident_bf = self.sb(es, "ident_bf", [128, 128], BF16)
        self.ident_f = self.sb(es, "ident_f", [128, 128], F32)
        self.r_const = S.res("const")
        rc = self.r_const
        S.op("pool", lambda e: e.memset(self.ident_f[:], 0.0), writes=[rc])
        S.op("pool", lambda e: e.affine_select(out=self.ident_f[:], in_=self.ident_f[:], pattern=[[-1, 128]],
                                               compare_op=ALU.not_equal, fill=1.0, base=0, channel_multiplier=1),
             reads=[rc], writes=[rc])
        S.op("pool", lambda e: e.tensor_copy(out=self.ident_bf[:], in_=self.ident_f[:]), reads=[rc], writes=[rc])
        self.banks = [es.enter_context(nc.psum_tensor("bank%d" % i, [128, 512], F32)) for i in range(8)]
        self.rbank = [S.res("bank%d" % i) for i in range(8)]
        self.r_uvb = S.res("uvb")
        self.phase1()
        S.barrier()
        self.phase2()
        S.barrier()

    def convert_uv_piece(self, i):
        S = self.S
        src = (self.uT, self.vC)[i // 16]
        off = (i // 16) * 1024
        j = i % 16
        R = 1024
        S.dma("pool", self.uvb[j * R:(j + 1) * R, off:off + 1024], src[j * R:(j + 1) * R, :], writes=[self.r_uvb], multipart=True)

    def phase1(self):
        nc, S = self.nc, self.S
        T = self.T
        TB = 128
        NB = T // TB
        with ExitStack() as es:
            sb = lambda name, shape, dtype: self.sb(es, name, shape, dtype)
            win = sb("win", [128, 8, INC], BF16)
            pa = sb("pa_sb", [128, 4, D], BF16)
            pb = sb("pb_sb", [128, 4, D], BF16)
            wo = sb("wo_sb", [128, 8, D], BF16)
            r_wA = S.res("w1A")
            r_wB = S.res("w1B")
            r_wC = S.res("w1C")
            r_wp = S.res("w1p")
            r_wo = S.res("w1o")
            for (c0, c1, rr) in ((0, 2048, r_wA), (2048, 3584, r_wB), (3584, INC, r_wC)):
                for k in range(8):
                    S.dma("pool", win[:, k, c0:c1], self.w_in[k * 128:(k + 1) * 128, c0:c1], writes=[rr], multipart=True)
            for k in range(4):
                S.dma("pool", pa[:, k, :], self.pa[k * 128:(k + 1) * 128, :], writes=[r_wp], multipart=True)
                S.dma("pool", pb[:, k, :], self.pb[k * 128:(k + 1) * 128, :], writes=[r_wp], multipart=True)
            for k in range(8):
                S.dma("pool", wo[:, k, :], self.wo[k * 128:(k + 1) * 128, :], writes=[r_wo], multipart=True)

            def r_w_for(c0):
                return r_wA if c0 < 2048 else (r_wB if c0 < 3584 else r_wC)
            g1bc = sb("g1bc_sb", [128, D], F32)
            gobc = sb("gobc_sb", [64, 512], F32)
            lbl = sb("lbl_sb", [64, 16], F32)
            convw = sb("convw_sb", [128, 12], F32)
            r_c = S.res("c1")
            S.dma("sp", g1bc[:], self.g1bc[:, :], writes=[r_c], multipart=True)
            S.dma("sp", gobc[:], self.gobc[:, :], writes=[r_c], multipart=True)
            S.dma("sp", lbl[:], self.lbl[:, :], writes=[r_c], multipart=True)
            S.dma("sp", convw[:], self.convw[:, :], writes=[r_c], multipart=True)
            maskT = sb("maskT", [64, 64], F32)
            m01 = sb("m01", [64, TB], F32)
            r_m = S.res("masks")
            S.op("pool", lambda e: e.memset(maskT[:], 1.0), writes=[r_m])
            S.op("pool", lambda e: e.affine_select(out=maskT[:], in_=maskT[:], pattern=[[1, 64]], compare_op=ALU.is_ge,
                                                   fill=0.0, base=0, channel_multiplier=-1), reads=[r_m], writes=[r_m])
            S.op("pool", lambda e: e.memset(m01[:], 1.0), writes=[r_m])
            S.op("pool", lambda e: e.memset(m01[:, 0:TB:64], 0.0), writes=[r_m])
            lbe = sb("lbe", [64, 16], F32)
            lbs = sb("lbs", [64, 8], F32)
            lb = sb("lb", [64, 8], F32)
            oml = sb("oml", [64, 8], F32)
            r_lb = S.res("lb")
            S.op("act", lambda e: e.activation(out=lbe[:], in_=lbl[:], func=AF.Exp), reads=[r_c], writes=[r_lb])
            S.op("dve", lambda e: e.tensor_tensor(out=lbs[:], in0=lbe[:, 0:8], in1=lbe[:, 8:16], op=ALU.add), reads=[r_lb], writes=[r_lb])
            S.op("dve", lambda e: e.reciprocal(out=lbs[:], in_=lbs[:]), reads=[r_lb], writes=[r_lb])
            S.op("dve", lambda e: e.tensor_tensor(out=lb[:], in0=lbe[:, 0:8], in1=lbs[:], op=ALU.mult), reads=[r_lb], writes=[r_lb])
            S.op("dve", lambda e: e.tensor_scalar(out=oml[:], in0=lb[:], scalar1=-1.0, scalar2=1.0, op0=ALU.mult, op1=ALU.add),
                 reads=[r_lb], writes=[r_lb])
            st = sb("st", [64, 8, 64], F32)
            stb = [sb("stb%d" % i, [64, 8, 64], BF16) for i in range(2)]
            r_st = S.res("st")
            r_stb = [S.res("stb0"), S.res("stb1")]
            S.op("dve", lambda e: e.memset(st[:], 0.0), writes=[r_st])
            S.op("dve", lambda e: e.memset(stb[0][:], 0.0), writes=[r_stb[0]])
            ubuf = sb("ubuf", [128, 4, TB + 2], F32)
            r_u = [S.res("u%d" % j) for j in range(4)]
            S.op("dve", lambda e: e.memset(ubuf[:], 0.0), writes=r_u)
            xin = [sb("xin%d" % i, [128, D], F32) for i in range(2)]
            r_xin = [S.res("xin0"), S.res("xin1")]
            ssq = sb("ssq", [128, 1], F32)
            rstd = sb("rstd", [128, 1], F32)
            r_ssq = S.res("ssq")
            hbs = [sb("hb%d" % i, [128, D], BF16) for i in range(2)]
            r_hbs = [S.res("hb0"), S.res("hb1")]
            hTs = [sb("hT%d" % i, [128, 8, TB], BF16) for i in range(2)]
            r_hTs = [S.res("hT0"), S.res("hT1")]
            thq = sb("thq", [64, 4, TB], BF16)
            sq = sb("sq", [64, 4, TB], BF16)
            fg = sb("fg", [64, 4, TB], F32)
            kk = sb("kk", [64, 4, TB], BF16)
            lf = sb("lf", [64, 4, TB], F32)
            bb = sb("bb", [64, 4, TB], F32)
            r_thq = [S.res("thq%d" % i) for i in range(4)]
            r_sq = [S.res("sq%d" % i) for i in range(4)]
            r_fg = [S.res("fg%d" % i) for i in range(4)]
            r_kk = [S.res("kk%d" % i) for i in range(4)]
            r_lf = [S.res("lf%d" % i) for i in range(4)]
            r_bb = [S.res("bb%d" % i) for i in range(4)]
            nh1 = sb("nh1", [128, 8], F32)
            r_nh1 = S.res("nh1")
            S.op("pool", lambda e: e.memset(nh1[:], -0.5), writes=[r_nh1])
            gobh = sb("gobh", [64, 512], F32)
            r_gobh = S.res("gobh")
            S.op("pool", lambda e: e.tensor_scalar(out=gobh[:], in0=gobc[:], scalar1=0.5, scalar2=None, op0=ALU.mult), reads=[r_c], writes=[r_gobh])
            fc1 = sb("fc1", [64, 8], F32)
            fc0 = sb("fc0", [64, 8], F32)
            S.op("dve", lambda e: e.tensor_scalar(out=fc1[:], in0=oml[:], scalar1=0.5, scalar2=None, op0=ALU.mult), reads=[r_lb], writes=[r_lb])
            S.op("dve", lambda e: e.tensor_tensor(out=fc0[:], in0=lb[:], in1=fc1[:], op=ALU.add), reads=[r_lb], writes=[r_lb])
            qT = sb("qT", [64, 8, TB], BF16)
            kT = sb("kT", [64, 8, TB], BF16)
            r_qT = [S.res("qT%d" % h) for h in range(8)]
            r_kT = [S.res("kT%d" % h) for h in range(8)]
            ebl = sb("ebl", [64, 8, TB // 64], F32)
            r_ebl = [S.res("ebl%d" % h) for h in range(8)]
            v_sb = sb("v_sb", [64, TB // 64, 512], BF16)
            r_v = [S.res("v%d" % c) for c in range(TB // 64)]
            gsg = sb("gsg", [64, TB // 64, 512], F32)
            sgt = sb("sgt", [64, TB // 64, 512], F32)
            r_gsg = [S.res("gsg%d" % c) for c in range(TB // 64)]
            cc_sb = sb("cc_sb", [128, 4, TB], F32)
            cb_sb = sb("cb_sb", [128, 4, TB], F32)
            acc = sb("acc", [128, 4, TB], F32)
            r_cc = [S.res("cc%d" % j) for j in range(4)]
            r_cb = [S.res("cb%d" % j) for j in range(4)]
            r_acc = [S.res("acc%d" % j) for j in range(4)]
            ybT = sb("ybT", [128, 4, TB], BF16)
            r_yb = [S.res("yb%d" % j) for j in range(4)]
            sgT = sb("sgT", [128, 16, TB], BF16)
            r_sg = [S.res("sgT%d" % j) for j in range(16)]
            scm = [sb("scm%d" % i, [64, 8, 64], BF16) for i in range(2)]
            r_scm = [S.res("scm0"), S.res("scm1")]
            ktm = [sb("ktm%d" % i, [64, 8, 64], BF16) for i in range(2)]
            r_ktm = [S.res("ktm0"), S.res("ktm1")]
            sttmp = sb("sttmp", [64, 8, 64], F32)
            r_sttmp = S.res("sttmp")
            sqo = sb("sqo", [64, 8, 64], F32)
            r_sqo = S.res("sqo")
            so = sb("so", [64, 8], F32)
            r_so = S.res("so")
            y1 = sb("y1", [64, 8, 64], F32)
            r_y1 = S.res("y1")
            ya = [sb("ya%d" % i, [64, 512], BF16) for i in range(2)]
            r_ya = [S.res("ya0"), S.res("ya1")]
            yaT = sb("yaT", [128, 4, TB], BF16)
            r_yaT = S.res("yaT")
            mT = sb("mT", [128, 8, TB], BF16)
            r_mT = [S.res("mT%d" % j) for j in range(8)]
            t1 = [sb("t1_%d" % i, [128, TB], F32) for i in range(2)]
            t2 = [sb("t2_%d" % i, [128, TB], F32) for i in range(2)]
            r_t1 = [S.res("t1_0"), S.res("t1_1")]
            r_t2 = [S.res("t2_0"), S.res("t2_1")]
            r_x2d = S.res("x2d")

            bankctr = [0]

            def nb():
                i = bankctr[0] % 8
                bankctr[0] += 1
                return self.banks[i], self.rbank[i]

            LN8 = math.log(0.125)
            NCH = TB // 64
            cctr = [0]

            LNQ = math.log(0.125 * 0.5)
            def head(tb):
                xi, rx = xin[tb % 2], r_xin[tb % 2]
                hb, r_hb = hbs[tb % 2], r_hbs[tb % 2]
                hT, r_hT = hTs[tb % 2], r_hTs[tb % 2]
                S.op("act", lambda e: e.activation(out=hb[:], in_=xi[:], func=AF.Square, accum_out=ssq[:]),
                     reads=[rx], writes=[r_hb, r_ssq])
                S.op("pool", lambda e: e.tensor_scalar(out=rstd[:], in0=ssq[:], scalar1=1.0 / D, scalar2=EPS, op0=ALU.mult, op1=ALU.add),
                     reads=[r_ssq], writes=[r_ssq])
                S.op("pool", lambda e: e.tensor_tensor(out=rstd[:], in0=rstd[:], in1=nh1[:, 0:1], op=ALU.pow), reads=[r_ssq, r_nh1], writes=[r_ssq])
                S.op("dve", lambda e: e.scalar_tensor_tensor(out=hb[:], in0=xi[:], scalar=rstd[:], in1=g1bc[:], op0=ALU.mult, op1=ALU.mult),
                     reads=[rx, r_ssq, r_c], writes=[r_hb])
                bk, rb = nb()
                bkb = bk[:].bitcast(BF16)
                for k in range(8):
                    S.op("pe", lambda e, k=k: e.transpose(out=bkb[:, k * 128:(k + 1) * 128], in_=hb[:, k * 128:(k + 1) * 128], identity=self.ident_bf[:]),
                         reads=[r_hb, self.r_const], writes=[rb])
                S.op("act", lambda e: e.activation(out=hT[:], in_=bkb.rearrange("p (k t) -> p k t", k=8), func=AF.Copy),
                     reads=[rb], writes=[r_hT])

            S.dma("sp", xin[0][:], self.x[0:TB, :], writes=[r_xin[0]])
            if NB > 1:
                S.dma("sp", xin[1][:], self.x[TB:2 * TB, :], writes=[r_xin[1]])
            head(0)
            for tb in range(NB):
                xi = xin[tb % 2]
                rx = r_xin[tb % 2]
                hT, r_hT = hTs[tb % 2], r_hTs[tb % 2]

                def proj_fm(c0, ncols, bk, rb):
                    for k in range(8):
                        S.op("pe", lambda e, k=k: e.matmul(bk[0:ncols, 0:TB], lhsT=win[:, k, c0:c0 + ncols], rhs=hT[:, k, :],
                                                            start=(k == 0), stop=(k == 7)),
                             reads=[r_hT, r_w_for(c0)], writes=[rb])

                for g in range(2):
                    for hi in range(4):
                        h = 4 * g + hi
                        bk, rb = nb()
                        proj_fm(h * 64, 64, bk, rb)
                        S.op("act", lambda e: e.activation(out=thq[:, hi, :], in_=bk[0:64, 0:TB], func=AF.Tanh, scale=0.5), reads=[rb], writes=[r_thq[hi]])
                        S.op("dve", lambda e: e.scalar_tensor_tensor(out=sq[:, hi, :], in0=thq[:, hi, :], scalar=1.0, in1=bk[0:64, 0:TB], op0=ALU.add, op1=ALU.mult),
                             reads=[r_thq[hi], rb], writes=[r_sq[hi]])
                    if g == 0:
                        for c in range(NCH):
                            bk, rb = nb()
                            for k in range(8):
                                S.op("pe", lambda e, k=k: e.matmul(bk[0:64, :], lhsT=hT[:, k, c * 64:(c + 1) * 64], rhs=win[:, k, 1536:2048],
                                                                    start=(k == 0), stop=(k == 7)), reads=[r_hT, r_wA], writes=[rb])
                            S.op("act", lambda e: e.activation(out=sgt[:, c, :], in_=bk[0:64, :], func=AF.Tanh, scale=0.5), reads=[rb], writes=[r_gsg[c]])
                            S.op("dve", lambda e: e.scalar_tensor_tensor(out=gsg[:, c, :], in0=sgt[:, c, :], scalar=1.0, in1=bk[0:64, :], op0=ALU.add, op1=ALU.mult),
                                 reads=[r_gsg[c], rb], writes=[r_gsg[c]])
                            S.op("dve", lambda e: e.tensor_tensor(out=gsg[:, c, :], in0=gsg[:, c, :], in1=gobh[:], op=ALU.mult),
                                 reads=[r_gsg[c], r_gobh], writes=[r_gsg[c]])
                    for hi in range(4):
                        h = 4 * g + hi
                        bk, rb = nb()
                        proj_fm(512 + h * 64, 64, bk, rb)
                        S.op("act", lambda e: e.activation(out=fg[:, hi, :], in_=bk[0:64, 0:TB], func=AF.Tanh, scale=0.5), reads=[rb], writes=[r_fg[hi]])
                        S.op("dve", lambda e: e.tensor_scalar(out=fg[:, hi, :], in0=fg[:, hi, :], scalar1=fc1[:, h:h + 1], scalar2=fc0[:, h:h + 1],
                                                              op0=ALU.mult, op1=ALU.add), reads=[r_fg[hi], r_lb], writes=[r_fg[hi]])
                        S.op("dve", lambda e: e.tensor_scalar(out=kk[:, hi, :], in0=fg[:, hi, :], scalar1=-1.0, scalar2=1.0, op0=ALU.mult, op1=ALU.add),
                             reads=[r_fg[hi]], writes=[r_kk[hi]])
                    if g == 0:
                        for c in range(NCH):
                            bk, rb = nb()
                            for k in range(8):
                                S.op("pe", lambda e, k=k: e.matmul(bk[0:64, :], lhsT=hT[:, k, c * 64:(c + 1) * 64], rhs=win[:, k, 1024:1536],
                                                                    start=(k == 0), stop=(k == 7)), reads=[r_hT, r_wA], writes=[rb])
                            S.op("act", lambda e: e.activation(out=v_sb[:, c, :], in_=bk[0:64, :], func=AF.Copy), reads=[rb], writes=[r_v[c]])
                    for hi in range(4):
                        S.op("act", lambda e: e.activation(out=lf[:, hi, :], in_=fg[:, hi, :], func=AF.Ln), reads=[r_fg[hi]], writes=[r_lf[hi]])
                    for hi in range(4):
                        S.op("dve", lambda e: e.tensor_tensor_scan(out=bb[:, hi, :], data0=m01[:], data1=lf[:, hi, :], initial=0.0, op0=ALU.mult, op1=ALU.add),
                             reads=[r_lf[hi], r_m], writes=[r_bb[hi]])
                    for hi in range(4):
                        h = 4 * g + hi
                        S.op("act", lambda e: e.activation(out=ebl[:, h, :], in_=bb[:, hi, 63:TB:64], func=AF.Exp), reads=[r_bb[hi]], writes=[r_ebl[h]])
                        S.op("act", lambda e: e.activation(out=lf[:, hi, :], in_=bb[:, hi, :], func=AF.Exp, bias=LNQ), reads=[r_bb[hi], r_lf[hi]], writes=[r_lf[hi]])
                        S.op("act", lambda e: e.activation(out=bb[:, hi, :], in_=bb[:, hi, :], func=AF.Exp, scale=-1.0), reads=[r_bb[hi]], writes=[r_bb[hi]])
                    for hi in range(4):
                        h = 4 * g + hi
                        S.op("dve", lambda e: e.tensor_tensor(out=qT[:, h, :], in0=sq[:, hi, :], in1=lf[:, hi, :], op=ALU.mult), reads=[r_sq[hi], r_lf[hi]], writes=[r_qT[h]])
                        S.op("dve", lambda e: e.tensor_tensor(out=kT[:, h, :], in0=kk[:, hi, :], in1=bb[:, hi, :], op=ALU.mult), reads=[r_kk[hi], r_bb[hi]], writes=[r_kT[h]])

                fillq = []

                def conv_group(j, which):
                    def f():
                        bk, rb = nb()
                        if which == 0:
                            proj_fm(2560 + j * 128, 128, bk, rb)
                            S.op("act", lambda e: e.activation(out=cc_sb[:, j, :], in_=bk[:, 0:TB], func=AF.Copy), reads=[rb], writes=[r_cc[j]])
                        elif which == 1:
                            proj_fm(3072 + j * 128, 128, bk, rb)
                            S.op("dve", lambda e: e.tensor_tensor(out=ubuf[:, j, 2:TB + 2], in0=bk[:, 0:TB], in1=cc_sb[:, j, :], op=ALU.mult),
                                 reads=[rb, r_cc[j]], writes=[r_u[j]])
                        else:
                            proj_fm(2048 + j * 128, 128, bk, rb)
                            S.op("act", lambda e: e.activation(out=cb_sb[:, j, :], in_=bk[:, 0:TB], func=AF.Copy), reads=[rb], writes=[r_cb[j]])
                            S.op("dve", lambda e: e.tensor_scalar(out=acc[:, j, :], in0=ubuf[:, j, 2:TB + 2], scalar1=convw[:, j * 3 + 2:j * 3 + 3],
                                                                  scalar2=None, op0=ALU.mult), reads=[r_u[j], r_c], writes=[r_acc[j]])
                            S.op("dve", lambda e: e.scalar_tensor_tensor(out=acc[:, j, :], in0=ubuf[:, j, 1:TB + 1], scalar=convw[:, j * 3 + 1:j * 3 + 2],
                                                                         in1=acc[:, j, :], op0=ALU.mult, op1=ALU.add),
                                 reads=[r_u[j], r_c, r_acc[j]], writes=[r_acc[j]])
                            S.op("dve", lambda e: e.scalar_tensor_tensor(out=acc[:, j, :], in0=ubuf[:, j, 0:TB], scalar=convw[:, j * 3:j * 3 + 1],
                                                                         in1=acc[:, j, :], op0=ALU.mult, op1=ALU.add),
                                 reads=[r_u[j], r_c, r_acc[j]], writes=[r_acc[j]])
                            S.op("dve", lambda e: e.tensor_tensor(out=ybT[:, j, :], in0=acc[:, j, :], in1=cb_sb[:, j, :], op=ALU.mult),
                                 reads=[r_acc[j], r_cb[j]], writes=[r_yb[j]])
                            S.op("dve", lambda e: e.tensor_copy(out=ubuf[:, j, 0:2], in_=ubuf[:, j, TB:TB + 2]), reads=[r_u[j]], writes=[r_u[j]])
                    return f

                def gate_group(j):
                    def f():
                        bk, rb = nb()
                        proj_fm(3584 + j * 128, 128, bk, rb)
                        S.op("act", lambda e: e.activation(out=sgT[:, j, :], in_=bk[:, 0:TB], func=AF.Tanh, scale=0.5), reads=[rb], writes=[r_sg[j]])
                    return f

                for j in range(4):
                    for which in range(3):
                        fillq.append(conv_group(j, which))
                for j in range(16):
                    fillq.append(gate_group(j))

                def fill(n):
                    for _ in range(n):
                        if fillq:
                            fillq.pop(0)()

                fill(4)
                for c in range(NCH):
                    g = cctr[0]
                    cctr[0] += 1
                    cs = slice(c * 64, (c + 1) * 64)
                    sc, rsc = scm[g % 2], r_scm[g % 2]
                    kt, rkt = ktm[g % 2], r_ktm[g % 2]
                    sb_cur, rsb_cur = stb[g % 2], r_stb[g % 2]
                    sb_nxt, rsb_nxt = stb[(g + 1) % 2], r_stb[(g + 1) % 2]
                    bkA, rbA = nb()
                    for h in range(8):
                        S.op("pe", lambda e, h=h: e.matmul(bkA[0:64, h * 64:(h + 1) * 64], lhsT=kT[:, h, cs], rhs=qT[:, h, cs], start=True, stop=True),
                             reads=[r_kT[h], r_qT[h]], writes=[rbA])
                    bkB, rbB = nb()
                    bkBb = bkB[:].bitcast(BF16)
                    for h in range(8):
                        S.op("pe", lambda e, h=h: e.transpose(out=bkBb[0:64, h * 64:(h + 1) * 64], in_=kT[:, h, cs], identity=self.ident_bf[0:64, 0:64]),
                             reads=[r_kT[h], self.r_const], writes=[rbB])
                    S.op("dve", lambda e: e.tensor_tensor(out=sc[:], in0=bkA[0:64, :].rearrange("p (h t) -> p h t", h=8),
                                                          in1=maskT[:].unsqueeze(1).broadcast_to([64, 8, 64]), op=ALU.mult),
                         reads=[rbA, r_m], writes=[rsc])
                    S.op("act", lambda e: e.activation(out=kt[:], in_=bkBb[0:64, 0:512].rearrange("p (h d) -> p h d", h=8), func=AF.Copy),
                         reads=[rbB], writes=[rkt])
                    fill(3)
                    bkC, rbC = nb()
                    for h in range(8):
                        S.op("pe", lambda e, h=h: e.matmul(bkC[0:64, h * 64:(h + 1) * 64], lhsT=kt[:, h, :], rhs=v_sb[:, c, h * 64:(h + 1) * 64], start=True, stop=True),
                             reads=[rkt, r_v[c]], writes=[rbC])
                    bkD, rbD = nb()
                    for h in range(8):
                        S.op("pe", lambda e, h=h: e.matmul(bkD[0:64, h * 64:(h + 1) * 64], lhsT=sc[:, h, :], rhs=v_sb[:, c, h * 64:(h + 1) * 64], start=True, stop=False),
                             reads=[rsc, r_v[c]], writes=[rbD])
                        S.op("pe", lambda e, h=h: e.matmul(bkD[0:64, h * 64:(h + 1) * 64], lhsT=qT[:, h, cs], rhs=sb_cur[:, h, :], start=False, stop=True),
                             reads=[r_qT[h], rsb_cur], writes=[rbD])
                    S.op("dve", lambda e: e.tensor_tensor(out=sttmp[:], in0=bkC[0:64, :].rearrange("p (h e) -> p h e", h=8), in1=st[:], op=ALU.add),
                         reads=[rbC, r_st], writes=[r_sttmp])
                    S.op("dve", lambda e: e.tensor_tensor(out=st[:], in0=sttmp[:], in1=ebl[:, :, c:c + 1].broadcast_to([64, 8, 64]), op=ALU.mult),
                         reads=[r_sttmp] + r_ebl, writes=[r_st])
                    S.op("dve", lambda e: e.tensor_copy(out=sb_nxt[:], in_=st[:]), reads=[r_st], writes=[rsb_nxt])
                    S.op("act", lambda e: e.activation(out=sqo[:], in_=bkD[0:64, :].rearrange("p (h e) -> p h e", h=8), func=AF.Square),
                         reads=[rbD], writes=[r_sqo])
                    fill(3)
                    S.op("dve", lambda e: e.tensor_reduce(out=so[:], in_=sqo[:], axis=AX.X, op=ALU.add), reads=[r_sqo], writes=[r_so])
                    S.op("pool", lambda e: e.tensor_scalar(out=so[:], in0=so[:], scalar1=1.0 / 64, scalar2=EPS, op0=ALU.mult, op1=ALU.add),
                         reads=[r_so], writes=[r_so])
                    S.op("pool", lambda e: e.tensor_tensor(out=so[:], in0=so[:], in1=nh1[0:64, :], op=ALU.pow), reads=[r_so, r_nh1], writes=[r_so])
                    fill(3)
                    S.op("dve", lambda e: e.tensor_tensor(out=y1[:], in0=bkD[0:64, :].rearrange("p (h e) -> p h e", h=8),
                                                          in1=so[:].unsqueeze(2).broadcast_to([64, 8, 64]), op=ALU.mult),
                         reads=[rbD, r_so], writes=[r_y1])
                    yac, ryac = ya[g % 2], r_ya[g % 2]
                    S.op("dve", lambda e: e.tensor_tensor(out=yac[:], in0=y1[:].rearrange("p h e -> p (h e)"), in1=gsg[:, c, :], op=ALU.mult),
                         reads=[r_y1, r_gsg[c]], writes=[ryac])
                    fill(3)
                    bkE, rbE = nb()
                    bkEb = bkE[:].bitcast(BF16)
                    for k in range(4):
                        S.op("pe", lambda e, k=k: e.transpose(out=bkEb[:, k * 64:(k + 1) * 64], in_=yac[:, k * 128:(k + 1) * 128], identity=self.ident_bf[0:64, 0:64]),
                             reads=[ryac, self.r_const], writes=[rbE])
                    S.op("act", lambda e: e.activation(out=yaT[:, :, cs], in_=bkEb[:, 0:256].rearrange("p (k t) -> p k t", k=4), func=AF.Copy),
                         reads=[rbE], writes=[r_yaT])
                fill(100)
                npiece = (32 + NB - 1) // NB
                for pi in range(tb * npiece, min(32, (tb + 1) * npiece)):
                    self.convert_uv_piece(pi)
                if tb + 1 < NB:
                    head(tb + 1)
                for j in range(8):
                    bkA, rbA = nb()
                    for k in range(4):
                        S.op("pe", lambda e, k=k: e.matmul(bkA[:, 0:TB], lhsT=pa[:, k, j * 128:(j + 1) * 128], rhs=yaT[:, k, :], start=(k == 0), stop=(k == 3)),
                             reads=[r_yaT, r_wp], writes=[rbA])
                    bkB, rbB = nb()
                    for k in range(4):
                        S.op("pe", lambda e, k=k: e.matmul(bkB[:, 0:TB], lhsT=pb[:, k, j * 128:(j + 1) * 128], rhs=ybT[:, k, :], start=(k == 0), stop=(k == 3)),
                             reads=r_yb + [r_wp], writes=[rbB])
                    a1, ra1 = t1[j % 2], r_t1[j % 2]
                    a2, ra2 = t2[j % 2], r_t2[j % 2]
                    S.op("dve", lambda e: e.scalar_tensor_tensor(out=a1[:], in0=sgT[:, j, :], scalar=1.0, in1=bkA[:, 0:TB], op0=ALU.add, op1=ALU.mult),
                         reads=[rbA, r_sg[j]], writes=[ra1])
                    S.op("dve", lambda e: e.scalar_tensor_tensor(out=a2[:], in0=sgT[:, 8 + j, :], scalar=1.0, in1=bkB[:, 0:TB], op0=ALU.add, op1=ALU.mult),
                         reads=[rbB, r_sg[8 + j]], writes=[ra2])
                    S.op("dve", lambda e: e.tensor_tensor(out=mT[:, j, :], in0=a1[:], in1=a2[:], op=ALU.add), reads=[ra1, ra2], writes=[r_mT[j]])
                for half in range(2):
                    bk, rb = nb()
                    for k in range(8):
                        S.op("pe", lambda e, k=k: e.matmul(bk[:, :], lhsT=mT[:, k, :], rhs=wo[:, k, half * 512:(half + 1) * 512], start=(k == 0), stop=(k == 7)),
                             reads=[r_mT[k], r_wo], writes=[rb])
                    S.op("dve", lambda e: e.scalar_tensor_tensor(out=xi[:, half * 512:(half + 1) * 512], in0=bk[:, :], scalar=0.5,
                                                                 in1=xi[:, half * 512:(half + 1) * 512], op0=ALU.mult, op1=ALU.add),
                         reads=[rb, rx], writes=[rx])
                S.dma("sp", self.x2s[tb * TB:(tb + 1) * TB, :], xi[:], reads=[rx], writes=[r_x2d], semres=rx)
                if tb + 2 < NB:
                    S.dma("sp", xi[:], self.x[(tb + 2) * TB:(tb + 3) * TB, :], writes=[rx])

    def phase2(self):
        nc, S = self.nc, self.S
        T = self.T
        TB = 256
        NB = T // TB
        idxw_d = self.idxw_d
        r_idxd = [S.res("idxd%d" % i) for i in range(T // 128)]
        with ExitStack() as es0:
            sb0 = lambda name, shape, dtype: self.sb(es0, name, shape, dtype)
            g2bc = sb0("g2bc_sb", [128, D], F32)
            gfbc = sb0("gfbc_sb", [128, D], F32)
            r_c = S.res("c2")
            S.dma("sp", g2bc[:], self.g2bc[:, :], writes=[r_c], multipart=True)
            S.dma("sp", gfbc[:], self.gfbc[:, :], writes=[r_c], multipart=True)
            iota_f = sb0("iota_f", [128, 128], F32)
            iota_bf = sb0("iota_bf", [128, 128], BF16)
            iotaKA = sb0("iotaKA", [128, 16, 16], F32)
            r_io = S.res("iota")
            S.op("pool", lambda e: e.iota(iota_f[:], pattern=[[1, 128]], base=0, channel_multiplier=0, allow_small_or_imprecise_dtypes=True), writes=[r_io])
            S.op("pool", lambda e: e.tensor_copy(out=iota_bf[:], in_=iota_f[:]), reads=[r_io], writes=[r_io])
            S.op("pool", lambda e: e.iota(iotaKA[:], pattern=[[0, 16], [1, 16]], base=0, channel_multiplier=0, allow_small_or_imprecise_dtypes=True), writes=[r_io])
            xs = sb0("xs", [128, D], F32)
            r_xs = S.res("xs")
            junk = sb0("junk2", [128, D], BF16)
            r_junk = S.res("junk2")
            ssq = sb0("ssq2", [128, 2], F32)
            rstd = sb0("rstd2", [128, 2], F32)
            r_ssq = S.res("ssq2")
            h2 = sb0("h2", [128, D], BF16)
            r_h2 = S.res("h2")
            h2T = [sb0("h2T%d" % i, [128, 8, TB], BF16) for i in range(2)]
            r_h2T = [S.res("h2T0"), S.res("h2T1")]

            def load_norm_tile(b, tt, bank_idx):
                hT, rhT = h2T[b % 2], r_h2T[b % 2]
                r0 = b * TB + tt * 128
                S.dma("pool", xs[:], self.x2s[r0:r0 + 128, :], writes=[r_xs])
                S.op("act", lambda e: e.activation(out=junk[:], in_=xs[:], func=AF.Square, accum_out=ssq[:, 0:1]),
                     reads=[r_xs], writes=[r_junk, r_ssq])
                S.op("dve", lambda e: e.tensor_scalar(out=rstd[:, 0:1], in0=ssq[:, 0:1], scalar1=1.0 / D, scalar2=EPS, op0=ALU.mult, op1=ALU.add),
                     reads=[r_ssq], writes=[r_ssq])
                S.op("act", lambda e: e.activation(out=rstd[:, 0:1], in_=rstd[:, 0:1], func=AF.Sqrt), reads=[r_ssq], writes=[r_ssq])
                S.op("dve", lambda e: e.reciprocal(out=rstd[:, 0:1], in_=rstd[:, 0:1]), reads=[r_ssq], writes=[r_ssq])
                S.op("dve", lambda e: e.scalar_tensor_tensor(out=h2[:], in0=xs[:], scalar=rstd[:, 0:1], in1=g2bc[:],
                                                             op0=ALU.mult, op1=ALU.mult), reads=[r_xs, r_ssq, r_c], writes=[r_h2])
                bk, rb = self.banks[bank_idx], self.rbank[bank_idx]
                bkb = bk[:].bitcast(BF16)
                for k in range(8):
                    S.op("pe", lambda e, k=k: e.transpose(out=bkb[:, k * 128:(k + 1) * 128], in_=h2[:, k * 128:(k + 1) * 128], identity=self.ident_bf[:]),
                         reads=[r_h2, self.r_const], writes=[rb])
                S.op("act", lambda e: e.activation(out=hT[:, :, tt * 128:(tt + 1) * 128], in_=bkb.rearrange("p (k t) -> p k t", k=8), func=AF.Copy),
                     reads=[rb], writes=[rhT])

            with ExitStack() as es:
                sb = lambda name, shape, dtype: self.sb(es, name, shape, dtype)
                wq = sb("wq_sb", [128, 8, 2048], BF16)
                keysT = sb("keysT_sb", [128, 16, 128], BF16)
                r_w = S.res("w2")
                for k in range(8):
                    S.dma("pool", wq[:, k, :], self.wq[k * 128:(k + 1) * 128, :], writes=[r_w], multipart=True)
                S.dma("pool", keysT[:].rearrange("p g n -> p (g n)"), self.keysT[:, :], writes=[r_w], multipart=True)
                qT = [sb("qT2_%d" % i, [128, 16, TB], BF16) for i in range(2)]
                r_qT = [[S.res("qT2_%d_%d" % (i, g)) for g in range(16)] for i in range(2)]
                s_sb = [sb("s_sb%d" % i, [128, 16, 128], F32) for i in range(4)]
                r_s = [[S.res("s%d_%d" % (i, g)) for g in range(16)] for i in range(4)]

                class KS:
                    pass

                def mkset(p):
                    k = KS()
                    n = lambda s: "%s_p%d" % (s, p)
                    k.s_tmp = sb(n("s_tmp"), [128, 16, 128], F32)
                    k.r_stmp = [S.res(n("stmp%d" % g)) for g in range(16)]
                    k.v16 = sb(n("v16"), [128, 16, 16], F32)
                    k.i16 = sb(n("i16"), [128, 16, 16], U32)
                    k.r_v16 = [S.res(n("v16_%d" % g)) for g in range(16)]
                    k.r_i16 = [S.res(n("i16_%d" % g)) for g in range(16)]
                    k.i16f = sb(n("i16f"), [128, 16, 16], F32)
                    k.r_i16f = S.res(n("i16f"))
                    k.cand = sb(n("cand"), [128, 8, 256], F32)
                    k.r_cand = [S.res(n("cand%d" % h)) for h in range(8)]
                    k.ts16 = sb(n("ts16"), [128, 8, 16], F32)
                    k.pos = sb(n("pos"), [128, 8, 16], U32)
                    k.r_ts = [S.res(n("ts%d" % h)) for h in range(8)]
                    k.r_pos = [S.res(n("pos%d" % h)) for h in range(8)]
                    k.au = sb(n("au"), [128, 8, 16], U32)
                    k.bu = sb(n("bu"), [128, 8, 16], U32)
                    k.af = sb(n("af"), [128, 8, 16], F32)
                    k.bf = sb(n("bf"), [128, 8, 16], F32)
                    k.r_ab = S.res(n("ab"))
                    k.sel = sb(n("sel"), [128, 3, 128], F32)
                    k.r_sel = S.res(n("sel"))
                    k.ew = sb(n("ew"), [128, 8, 16], F32)
                    k.zz = sb(n("zz"), [128, 8], F32)
                    k.r_ew = S.res(n("ew"))
                    k.stg = sb(n("stg"), [128, 3, 128], BF16)
                    k.r_stg = S.res(n("stg"))
                    return k

                ksets = [mkset(0), mkset(1), mkset(2)]
                bctr = [0]

                def nb():
                    i = bctr[0] % 8
                    bctr[0] += 1
                    return self.banks[i], self.rbank[i]

                def stage_Q(b):
                    for tt in range(2):
                        i = bctr[0] % 8
                        bctr[0] += 1
                        load_norm_tile(b, tt, i)
                    hT, rhT = h2T[b % 2], r_h2T[b % 2]
                    for qc in range(16):
                        bk, rb = nb()
                        for k in range(8):
                            S.op("pe", lambda e, k=k: e.matmul(bk[:, 0:TB], lhsT=wq[:, k, qc * 128:(qc + 1) * 128], rhs=hT[:, k, :],
                                                                start=(k == 0), stop=(k == 7)), reads=[rhT, r_w], writes=[rb])
                        S.op("act", lambda e: e.activation(out=qT[b % 2][:, qc, :], in_=bk[:, 0:TB], func=AF.Copy), reads=[rb], writes=[r_qT[b % 2][qc]])

                def stage_S(b, tt, si):
                    tsl = slice(tt * 128, (tt + 1) * 128)
                    ss, rs = s_sb[si], r_s[si]
                    for g0 in range(0, 16, 4):
                        bk, rb = nb()
                        for gi in range(4):
                            g = g0 + gi
                            j, h = g // 8, g % 8
                            S.op("pe", lambda e: e.matmul(bk[:, gi * 128:(gi + 1) * 128], lhsT=qT[b % 2][:, h * 2 + j, tsl], rhs=keysT[:, g, :],
                                                          start=True, stop=True), reads=[r_qT[b % 2][h * 2 + j], r_w], writes=[rb])
                        S.op("act", lambda e: e.activation(out=ss[:, g0:g0 + 4, :], in_=bk[:].rearrange("p (g n) -> p g n", g=4), func=AF.Copy),
                             reads=[rb], writes=rs[g0:g0 + 4])

                def stage_K(n, slot):
                    si = n % 4
                    ss, rs = s_sb[si], r_s[si]
                    K = ksets[slot]
                    v16, i16, s_tmp, cand, ts16, pos = K.v16, K.i16, K.s_tmp, K.cand, K.ts16, K.pos
                    for g in range(16):
                        S.op("dve", lambda e: e.max(out=v16[:, g, 0:8], in_=ss[:, g, :]), reads=[rs[g]], writes=[K.r_v16[g]])
                    yield
                    for g in range(16):
                        S.op("dve", lambda e: e.max_index(out=i16[:, g, 0:8], in_max=v16[:, g, 0:8], in_values=ss[:, g, :]),
                             reads=[rs[g], K.r_v16[g]], writes=[K.r_i16[g]])
                    yield
                    for g in range(16):
                        S.op("dve", lambda e: e.match_replace(out=s_tmp[:, g, :], in_to_replace=v16[:, g, 0:8], in_values=ss[:, g, :], imm_value=NEG),
                             reads=[rs[g], K.r_v16[g]], writes=[K.r_stmp[g]])
                    yield
                    for g in range(16):
                        S.op("dve", lambda e: e.max(out=v16[:, g, 8:16], in_=s_tmp[:, g, :]), reads=[K.r_stmp[g]], writes=[K.r_v16[g]])
                    yield
                    for g in range(16):
                        S.op("dve", lambda e: e.max_index(out=i16[:, g, 8:16], in_max=v16[:, g, 8:16], in_values=s_tmp[:, g, :]),
                             reads=[K.r_stmp[g], K.r_v16[g]], writes=[K.r_i16[g]])
                    yield
                    S.op("act", lambda e: e.activation(out=K.i16f[:], in_=i16[:], func=AF.Copy), reads=K.r_i16, writes=[K.r_i16f])
                    S.op("pool", lambda e: e.tensor_tensor(out=cand[:].rearrange("p h (a b) -> p h a b", a=16),
                                                          in0=v16[:, 0:8, :].unsqueeze(3).broadcast_to([128, 8, 16, 16]),
                                                          in1=v16[:, 8:16, :].unsqueeze(2).broadcast_to([128, 8, 16, 16]), op=ALU.add),
                         reads=K.r_v16, writes=K.r_cand)
                    yield
                    ctmp = s_tmp[:].rearrange("p g n -> p (g n)").rearrange("p (h c) -> p h c", h=8)
                    for h in range(8):
                        S.op("dve", lambda e: e.max(out=ts16[:, h, 0:8], in_=cand[:, h, :]), reads=[K.r_cand[h]], writes=[K.r_ts[h]])
                    yield
                    for h in range(8):
                        S.op("dve", lambda e: e.max_index(out=pos[:, h, 0:8], in_max=ts16[:, h, 0:8], in_values=cand[:, h, :]),
                             reads=[K.r_cand[h], K.r_ts[h]], writes=[K.r_pos[h]])
                    yield
                    for h in range(8):
                        S.op("dve", lambda e: e.match_replace(out=ctmp[:, h, :], in_to_replace=ts16[:, h, 0:8], in_values=cand[:, h, :], imm_value=NEG),
                             reads=[K.r_cand[h], K.r_ts[h]], writes=K.r_stmp[2 * h:2 * h + 2])
                    yield
                    for h in range(8):
                        S.op("dve", lambda e: e.max(out=ts16[:, h, 8:16], in_=ctmp[:, h, :]), reads=K.r_stmp[2 * h:2 * h + 2], writes=[K.r_ts[h]])
                    yield
                    for h in range(8):
                        S.op("dve", lambda e: e.max_index(out=pos[:, h, 8:16], in_max=ts16[:, h, 8:16], in_values=ctmp[:, h, :]),
                             reads=K.r_stmp[2 * h:2 * h + 2] + [K.r_ts[h]], writes=[K.r_pos[h]])
                    yield
                    S.op("dve", lambda e: e.tensor_scalar(out=K.au[:], in0=pos[:], scalar1=4, scalar2=None, op0=ALU.logical_shift_right),
                         reads=K.r_pos, writes=[K.r_ab])
                    S.op("dve", lambda e: e.tensor_scalar(out=K.bu[:], in0=pos[:], scalar1=15, scalar2=None, op0=ALU.bitwise_and),
                         reads=K.r_pos, writes=[K.r_ab])
                    S.op("pool", lambda e: e.tensor_tensor(out=K.ew[:], in0=ts16[:], in1=ts16[:, :, 0:1].broadcast_to([128, 8, 16]), op=ALU.subtract),
                         reads=K.r_ts, writes=[K.r_ew])
                    S.op("act", lambda e: e.activation(out=K.ew[:], in_=K.ew[:], func=AF.Exp), reads=[K.r_ew], writes=[K.r_ew])
                    yield
                    S.op("act", lambda e: e.activation(out=K.af[:], in_=K.au[:], func=AF.Copy), reads=[K.r_ab], writes=[K.r_ab])
                    S.op("act", lambda e: e.activation(out=K.bf[:], in_=K.bu[:], func=AF.Copy), reads=[K.r_ab], writes=[K.r_ab])
                    yield
                    eq = ss[:].rearrange("p g n -> p (g n)").rearrange("p (h k a) -> p h k a", h=8, k=16)
                    for which, sel_ab, joff in ((0, K.af, 0), (1, K.bf, 8)):
                        S.op("dve", lambda e: e.tensor_tensor(out=eq, in0=sel_ab[:].unsqueeze(3).broadcast_to([128, 8, 16, 16]),
                                                              in1=iotaKA[:].unsqueeze(1).broadcast_to([128, 8, 16, 16]), op=ALU.is_equal),
                             reads=[K.r_ab, r_io], writes=rs)
                        yield
                        S.op("pool", lambda e: e.tensor_tensor(out=eq, in0=eq, in1=K.i16f[:, joff:joff + 8, :].unsqueeze(2).broadcast_to([128, 8, 16, 16]),
                                                               op=ALU.mult), reads=rs + [K.r_i16f], writes=rs)
                        yield
                        S.op("dve", lambda e: e.tensor_reduce(out=K.sel[:, which, :].rearrange("p (h k) -> p h k", h=8), in_=eq, axis=AX.X, op=ALU.add),
                             reads=rs, writes=[K.r_sel])
                        yield
                    S.op("dve", lambda e: e.tensor_reduce(out=K.zz[:], in_=K.ew[:], axis=AX.X, op=ALU.add), reads=[K.r_ew], writes=[K.r_ew])
                    yield
                    S.op("dve", lambda e: e.reciprocal(out=K.zz[:], in_=K.zz[:]), reads=[K.r_ew], writes=[K.r_ew])
                    yield
                    S.op("pool", lambda e: e.tensor_tensor(out=K.sel[:, 2, :].rearrange("p (h k) -> p h k", h=8), in0=K.ew[:],
                                                          in1=K.zz[:].unsqueeze(2).broadcast_to([128, 8, 16]), op=ALU.mult),
                         reads=[K.r_ew], writes=[K.r_sel])
                    yield
                    bk, rb = nb()
                    for i in range(3):
                        S.op("pe", lambda e, i=i: e.transpose(out=bk[:, i * 128:(i + 1) * 128], in_=K.sel[:, i, :], identity=self.ident_f[:]),
                             reads=[K.r_sel, self.r_const], writes=[rb])
                    yield
                    S.op("act", lambda e: e.activation(out=K.stg[:], in_=bk[:, 0:384].rearrange("p (i t) -> p i t", i=3), func=AF.Copy),
                         reads=[rb], writes=[K.r_stg])
                    S.dma("sp", idxw_d[:, :, n * 128:(n + 1) * 128], K.stg[:], reads=[K.r_stg], writes=[r_idxd[n]], semres=K.r_stg)

                tiles = [(b, tt) for b in range(NB) for tt in range(2)]
                s_done = [0]

                def ensure_S(upto):
                    while s_done[0] < min(upto, len(tiles)):
                        b, tt = tiles[s_done[0]]
                        if tt == 0:
                            stage_Q(b)
                        stage_S(b, tt, s_done[0] % 4)
                        s_done[0] += 1

                free_slots = [0, 1, 2]
                active = {}
                next_i = 0
                while next_i < len(tiles) or active:
                    while free_slots and next_i < len(tiles):
                        slot = free_slots.pop(0)
                        ensure_S(next_i + 2)
                        active[slot] = stage_K(next_i, slot)
                        next_i += 1
                    for slot in list(active):
                        try:
                            next(active[slot])
                        except StopIteration:
                            del active[slot]
                            free_slots.append(slot)
            S.barrier()
            with ExitStack() as es:
                sb = lambda name, shape, dtype: self.sb(es, name, shape, dtype)
                Gs = [sb("Gs%d" % i, [128, TB, 128], BF16) for i in range(2)]
                r_Gs = [[S.res("Gs%d_%d" % (i, j)) for j in range(TB // 4)] for i in range(2)]
                NU = 8
                uvslot = [sb("uvslot%d" % i, [128, 2048], BF16) for i in range(NU)]
                r_uv = [S.res("uv%d" % i) for i in range(NU)]
                idxb = [sb("idxb%d" % i, [128, 3, TB], BF16) for i in range(2)]
                r_idxb = [S.res("idxb0"), S.res("idxb1")]
                ga = [sb("ga%d" % i, [128, TB], BF16) for i in range(2)]
                gaT = [sb("gaT%d" % i, [128, TB], BF16) for i in range(2)]
                r_ga = [S.res("ga0"), S.res("ga1")]
                r_gaT = [S.res("gaT0"), S.res("gaT1")]
                x2b = sb("x2b", [128, 2, D], F32)
                r_x2b = S.res("x2b")
                r_yd = S.res("yd")
                steps = [(b, c) for b in range(NB) for c in range(128)]
                PF = NU - 1

                def issue_uv(i):
                    if i >= len(steps):
                        return
                    _, c = steps[i]
                    S.dma("sp", uvslot[i % NU][:], self.uvb[c * 128:(c + 1) * 128, :], reads=[self.r_uvb], writes=[r_uv[i % NU]])

                NL = 4
                L1 = [sb("L1_%d" % i, [128, 4, 128], BF16) for i in range(NL)]
                L2w = [sb("L2w_%d" % i, [128, 4, 128], BF16) for i in range(NL)]
                r_L1 = [S.res("L1_%d" % i) for i in range(NL)]
                r_L2w = [S.res("L2w_%d" % i) for i in range(NL)]
                nh = sb("nh", [128, 1], F32)
                r_nh = S.res("nh")
                S.op("pool", lambda e: e.memset(nh[:], -0.5), writes=[r_nh])
                ssqx = sb("ssqx", [128, 4], F32)
                rstx = sb("rstx", [128, 4], F32)
                r_sq = [S.res("ssqx%d" % i) for i in range(4)]
                pending = {}
                r_aslot = [S.res("aslot%d" % i) for i in range(4)]

                def at(step, fn):
                    pending.setdefault(step, []).append(fn)

                def load_idx(bb):
                    S.dma("pool", idxb[bb % 2][:], idxw_d[:, :, bb * TB:(bb + 1) * TB], reads=r_idxd[bb * 2:bb * 2 + 2], writes=[r_idxb[bb % 2]])

                def rstd_ops(col, s):
                    at(s, lambda: S.op("pool", lambda e: e.tensor_scalar(out=rstx[:, col:col + 1], in0=ssqx[:, col:col + 1], scalar1=1.0 / D, scalar2=EPS,
                                                                          op0=ALU.mult, op1=ALU.add), reads=[r_sq[col]], writes=[r_sq[col]]))
                    at(s + 2, lambda: S.op("pool", lambda e: e.tensor_tensor(out=rstx[:, col:col + 1], in0=rstx[:, col:col + 1], in1=nh[:], op=ALU.pow),
                                           reads=[r_sq[col], r_nh], writes=[r_sq[col]]))

                def sched_load_norm(bb, tt, s):
                    hT, rhT = h2T[bb % 2], r_h2T[bb % 2]
                    r0 = bb * TB + tt * 128
                    col = tt
                    at(s, lambda: S.dma("pool", xs[:], self.x2s[r0:r0 + 128, :], writes=[r_xs]))
                    at(s + 3, lambda: S.op("act", lambda e: e.activation(out=junk[:], in_=xs[:], func=AF.Square, accum_out=ssqx[:, col:col + 1]),
                                           reads=[r_xs], writes=[r_junk, r_sq[col]]))
                    rstd_ops(col, s + 5)
                    at(s + 9, lambda: S.op("dve", lambda e: e.scalar_tensor_tensor(out=h2[:], in0=xs[:], scalar=rstx[:, col:col + 1], in1=g2bc[:],
                                                                                    op0=ALU.mult, op1=ALU.mult), reads=[r_xs, r_sq[col], r_c], writes=[r_h2]))

                    def tr():
                        bk, rb = self.banks[7], self.rbank[7]
                        bkb = bk[:].bitcast(BF16)
                        for k in range(8):
                            S.op("pe", lambda e, k=k: e.transpose(out=bkb[:, k * 128:(k + 1) * 128], in_=h2[:, k * 128:(k + 1) * 128], identity=self.ident_bf[:]),
                                 reads=[r_h2, self.r_const], writes=[rb])
                    at(s + 11, tr)

                    def ev():
                        bk, rb = self.banks[7], self.rbank[7]
                        bkb = bk[:].bitcast(BF16)
                        S.op("act", lambda e: e.activation(out=hT[:, :, tt * 128:(tt + 1) * 128], in_=bkb.rearrange("p (k t) -> p k t", k=8), func=AF.Copy),
                             reads=[rb], writes=[rhT])
                    at(s + 13, ev)

                def sched_g_item(bb, j, s):
                    li = j % NL
                    ib, rib = idxb[bb % 2], r_idxb[bb % 2]

                    def dv():
                        for q in range(4):
                            t = 4 * j + q
                            S.op("dve", lambda e: e.tensor_scalar(out=L1[li][:, q, :], in0=iota_bf[:], scalar1=ib[:, 0, t:t + 1], scalar2=None, op0=ALU.is_equal),
                                 reads=[r_io, rib], writes=[r_L1[li]])
                            S.op("dve", lambda e: e.tensor_scalar(out=L2w[li][:, q, :], in0=iota_bf[:], scalar1=ib[:, 1, t:t + 1], scalar2=ib[:, 2, t:t + 1],
                                                                  op0=ALU.is_equal, op1=ALU.mult), reads=[r_io, rib], writes=[r_L2w[li]])

                    def pe():
                        bk, rb = self.banks[7], self.rbank[7]
                        for q in range(4):
                            S.op("pe", lambda e: e.matmul(bk[:, q * 128:(q + 1) * 128], lhsT=L1[li][:, q, :], rhs=L2w[li][:, q, :], start=True, stop=True),
                                 reads=[r_L1[li], r_L2w[li]], writes=[rb])

                    def ac():
                        bk, rb = self.banks[7], self.rbank[7]
                        S.op("act", lambda e: e.activation(out=Gs[bb % 2][:, 4 * j:4 * j + 4, :], in_=bk[:].rearrange("p (q n) -> p q n", q=4), func=AF.Copy),
                             reads=[rb], writes=[r_Gs[bb % 2][j]])
                    at(s, dv)
                    at(s + 3, pe)
                    at(s + 4, ac)

                def sched_next_block(bb, s0):
                    at(s0, lambda: load_idx(bb))
                    sched_load_norm(bb, 0, s0)
                    sched_load_norm(bb, 1, s0 + 12)
                    for j in range(TB // 4):
                        sched_g_item(bb, j, s0 + 28 + (j * 3) // 2)

                def emit_A(b, c):
                    i = b * 128 + c
                    us, rus = uvslot[i % NU][:, 0:1024], r_uv[i % NU]
                    sl = c % 3
                    bk, rb = self.banks[4 + sl], self.rbank[4 + sl]
                    co = 0
                    for k in range(8):
                        S.op("pe", lambda e, k=k: e.matmul(bk[:, co:co + TB], lhsT=us[:, k * 128:(k + 1) * 128], rhs=h2T[b % 2][:, k, :], start=(k == 0), stop=(k == 7)),
                             reads=[rus, r_h2T[b % 2]], writes=[rb])

                def emit_rest(b, c):
                    i = b * 128 + c
                    vs, rvs = uvslot[i % NU][:, 1024:2048], r_uv[i % NU]
                    sl = c % 3
                    bk, rb = self.banks[4 + sl], self.rbank[4 + sl]
                    co = 0
                    gg, rgg = ga[i % 2], r_ga[i % 2]
                    S.op("act", lambda e: e.activation(out=gg[:], in_=bk[:, co:co + TB], func=AF.Gelu), reads=[rb], writes=[rgg])
                    gt, rgt = gaT[i % 2], r_gaT[i % 2]
                    S.op("dve", lambda e: e.tensor_tensor(out=gt[:], in0=gg[:], in1=Gs[b % 2][:, :, c], op=ALU.mult), reads=[rgg] + r_Gs[b % 2], writes=[rgt])
                    for tt in range(2):
                        for half in range(2):
                            ai = tt * 2 + half
                            S.op("pe", lambda e: e.matmul(self.banks[ai][:, :], lhsT=gt[:, tt * 128:(tt + 1) * 128], rhs=vs[:, half * 512:(half + 1) * 512],
                                                          start=(c == 0), stop=(c == 127)), reads=[rgt, rvs], writes=[self.rbank[ai]])
                    issue_uv(i + NU)

                def final_evac(b):
                    for tt in range(2):
                        for half in range(2):
                            ai = tt * 2 + half
                            S.op("dve", lambda e: e.tensor_tensor(out=x2b[:, tt, half * 512:(half + 1) * 512], in0=self.banks[ai][:, :],
                                                                  in1=x2b[:, tt, half * 512:(half + 1) * 512], op=ALU.add),
                                 reads=[self.rbank[ai], r_x2b], writes=[r_x2b])

                def sched_final_norm(b, s):
                    for tt in range(2):
                        col = 2 + tt
                        s1 = s + 2 + tt * 3
                        at(s1, lambda tt=tt, col=col: S.op("act", lambda e: e.activation(out=junk[:], in_=x2b[:, tt, :], func=AF.Square, accum_out=ssqx[:, col:col + 1]),
                                                           reads=[r_x2b], writes=[r_junk, r_sq[col]]))
                        rstd_ops(col, s1 + 2)
                        at(s1 + 7, lambda tt=tt, col=col: S.op("dve", lambda e: e.scalar_tensor_tensor(out=x2b[:, tt, :], in0=x2b[:, tt, :], scalar=rstx[:, col:col + 1], in1=gfbc[:],
                                                                                                    op0=ALU.mult, op1=ALU.mult), reads=[r_x2b, r_sq[col], r_c], writes=[r_x2b]))
                    at(s + 15, lambda: S.dma("pool", self.y[b * TB:(b + 1) * TB, :].rearrange("(tt p) d -> p tt d", p=128), x2b[:], reads=[r_x2b], writes=[r_yd], semres=r_x2b))

                def run_pending(step):
                    for fn in pending.pop(step, []):
                        fn()

                for i in range(NU):
                    issue_uv(i)
                sched_next_block(0, -200)
                for s in sorted(pending):
                    run_pending(s)
                for b in range(NB):
                    s0 = b * 128
                    if b + 1 < NB:
                        sched_next_block(b + 1, s0)
                    at(s0 + 40, lambda b=b: S.dma("pool", x2b[:], self.x2s[b * TB:(b + 1) * TB, :].rearrange("(tt p) d -> p tt d", p=128), writes=[r_x2b]))
                    emit_A(b, 0)
                    emit_A(b, 1)
                    for c in range(128):
                        if c + 2 < 128:
                            emit_A(b, c + 2)
                        emit_rest(b, c)
                        run_pending(s0 + c)
                    final_evac(b)
                    sched_final_norm(b, s0 + 128)
                for s in sorted(pending):
                    run_pending(s)
            S.barrier()


def host_layout(inputs):
    f = lambda a: np.ascontiguousarray(np.asarray(a, dtype=np.float32))
    w = {}
    w["w_in"] = f(inputs["w_in"][0])
    w["pa"] = f(inputs["w_branch_hg"][0])
    w["pb"] = f(inputs["w_branch_conv"][0])
    w["wo"] = f(inputs["w_out"][0])
    w["wq"] = f(inputs["peer_w_query"][0])
    k1 = np.asarray(inputs["peer_keys1"][0])
    k2 = np.asarray(inputs["peer_keys2"][0])
    ks = np.stack([k1, k2], 0)
    w["keysT"] = f(ks.transpose(3, 0, 1, 2).reshape(128, 16 * 128))
    U = np.asarray(inputs["peer_u"][0])
    w["uT"] = f(U.reshape(128, 128, 8, 128).transpose(1, 3, 2, 0).reshape(128 * 128, 1024))
    V = np.asarray(inputs["peer_v"][0])
    w["vC"] = f(V.reshape(128, 128, 1024).transpose(1, 0, 2).reshape(128 * 128, 1024))
    w["g1bc"] = f(np.broadcast_to(np.asarray(inputs["norm_mix_g"][0])[None, :], (128, D)))
    w["g2bc"] = f(np.broadcast_to(np.asarray(inputs["norm_ffn_g"][0])[None, :], (128, D)))
    w["gfbc"] = f(np.broadcast_to(np.asarray(inputs["norm_final_g"])[None, :], (128, D)))
    w["gobc"] = f(np.broadcast_to(np.asarray(inputs["hg_out_norm_g"][0])[None, :], (64, 512)))
    lg = np.asarray(inputs["hg_lb_logits"])
    w["lbl"] = f(lg.reshape(2, 8, 64).transpose(2, 0, 1).reshape(64, 16))
    cw = np.asarray(inputs["conv_w"][0])
    w["convw"] = f(cw.reshape(3, 4, 128).transpose(2, 1, 0).reshape(128, 12))
    return w


_PROG_CACHE = {}


def run(inputs, T, ncores, dbg=None):
    key = (T, dbg)
    if key not in _PROG_CACHE:
        _PROG_CACHE[key] = Prog(T, dbg)
    prog = _PROG_CACHE[key]
    w = host_layout(inputs)
    x = np.asarray(inputs["x"], dtype=np.float32)
    in_maps = []
    for c in range(ncores):
        m = dict(w)
        m["x"] = np.ascontiguousarray(x[c, :T, :])
        in_maps.append(m)
    res = run_bass_kernel_spmd(prog.nc, in_maps, core_ids=list(range(ncores)))
    return res


def kernel(**inputs):
    res = run(inputs, 4096, 8)
    out = np.stack([np.asarray(r["y"], dtype=np.float32) for r in res.results], 0)
    return out
```

```python
import math
from contextlib import ExitStack
import numpy as np
import concourse.bass as bass
import concourse.mybir as mybir
from concourse.bass_utils import run_bass_kernel_spmd

F32 = mybir.dt.float32
BF16 = mybir.dt.bfloat16
U32 = mybir.dt.uint32
AF = mybir.ActivationFunctionType
ALU = mybir.AluOpType
AX = mybir.AxisListType

D = 1024
INC = 5632
EPS = 1e-6
NEG = -1.0e30


class Res:
    __slots__ = ("name", "w", "readers", "dreads", "dsem", "dcount")

    def __init__(self, name):
        self.name = name
        self.w = None
        self.readers = {}
        self.dreads = []
        self.dsem = None
        self.dcount = 0


class Eng:
    def __init__(self, name, obj, sem):
        self.name = name
        self.obj = obj
        self.sem = sem
        self.count = 0
        self.waited = {}


class Sched:
    def __init__(self, nc, es):
        self.nc = nc
        self.es = es
        self.engs = {}
        for name, obj in (("pe", nc.tensor), ("act", nc.scalar), ("dve", nc.vector),
                          ("pool", nc.gpsimd), ("sp", nc.sync)):
            sem = es.enter_context(nc.semaphore("sem_" + name))
            self.engs[name] = Eng(name, obj, sem)
        self.dsems = []
        self.nres = 0

    def res(self, name=None):
        self.nres += 1
        return Res(name or f"r{self.nres}")

    def _wait(self, eng, sem, val):
        key = id(sem)
        if eng.waited.get(key, 0) < val:
            eng.obj.wait_ge(sem, val)
            eng.waited[key] = val

    def _deps(self, engname, reads, writes, multipart=False):
        deps = []
        for r in reads:
            if r.w is not None:
                deps.append(r.w)
        for w in writes:
            if w.w is not None:
                same_pe = (w.w[0] == 'e' and w.w[1] == engname == 'pe')
                same_multipart = (multipart and w.w[0] == 'd' and w.dsem is not None and w.w[1] is w.dsem)
                if not (same_pe or same_multipart):
                    deps.append(w.w)
            for en, idx in w.readers.items():
                deps.append(('e', en, idx))
            for sem, val in w.dreads:
                deps.append(('d', sem, val))
        return deps

    def _emit_waits(self, eng, deps):
        for d in deps:
            if d[0] == 'e':
                self._wait(eng, self.engs[d[1]].sem, d[2])
            else:
                self._wait(eng, d[1], d[2])

    def op(self, engname, fn, reads=(), writes=()):
        eng = self.engs[engname]
        self._emit_waits(eng, self._deps(engname, reads, writes))
        inst = fn(eng.obj)
        eng.count += 1
        inst.then_inc(eng.sem, 1)
        for r in reads:
            r.readers[engname] = eng.count
        for w in writes:
            w.w = ('e', engname, eng.count)
            w.readers = {}
            w.dreads = []
        return inst

    def dma(self, engname, out, in_, reads=(), writes=(), semres=None, multipart=False, **kw):
        eng = self.engs[engname]
        self._emit_waits(eng, self._deps(engname, reads, writes, multipart))
        sr = semres if semres is not None else (writes[0] if writes else reads[0])
        if sr.dsem is None:
            sr.dsem = self.es.enter_context(self.nc.semaphore("dsem_%s_%d" % (sr.name, len(self.dsems))))
            self.dsems.append(sr)
        sr.dcount += 16
        inst = eng.obj.dma_start(out=out, in_=in_, **kw)
        inst.then_inc(sr.dsem, 16)
        for r in reads:
            r.dreads.append((sr.dsem, sr.dcount))
        for w in writes:
            w.w = ('d', sr.dsem, sr.dcount)
            w.readers = {}
            w.dreads = []
        return inst

    def sync_self(self, engname):
        eng = self.engs[engname]
        if eng.count > 0:
            self._wait(eng, eng.sem, eng.count)

    def barrier(self):
        for e in self.engs.values():
            for f in self.engs.values():
                if f is not e and f.count > 0:
                    self._wait(e, f.sem, f.count)
            for sr in self.dsems:
                if sr.dcount > 0:
                    self._wait(e, sr.dsem, sr.dcount)


class Prog:
    def __init__(self, T, dbg=None):
        self.T = T
        self.dbg = dbg
        nc = bass.Bass("TRN2", target_bir_lowering=False)
        self.nc = nc
        dt = nc.dram_tensor
        self.x = dt("x", [T, D], F32, kind="ExternalInput").ap()
        self.w_in = dt("w_in", [D, INC], F32, kind="ExternalInput").ap()
        self.pa = dt("pa", [512, D], F32, kind="ExternalInput").ap()
        self.pb = dt("pb", [512, D], F32, kind="ExternalInput").ap()
        self.wo = dt("wo", [D, D], F32, kind="ExternalInput").ap()
        self.wq = dt("wq", [D, 2048], F32, kind="ExternalInput").ap()
        self.keysT = dt("keysT", [128, 16 * 128], F32, kind="ExternalInput").ap()
        self.uT = dt("uT", [128 * 128, 1024], F32, kind="ExternalInput").ap()
        self.vC = dt("vC", [128 * 128, 1024], F32, kind="ExternalInput").ap()
        self.g1bc = dt("g1bc", [128, D], F32, kind="ExternalInput").ap()
        self.g2bc = dt("g2bc", [128, D], F32, kind="ExternalInput").ap()
        self.gfbc = dt("gfbc", [128, D], F32, kind="ExternalInput").ap()
        self.gobc = dt("gobc", [64, 512], F32, kind="ExternalInput").ap()
        self.lbl = dt("lbl", [64, 16], F32, kind="ExternalInput").ap()
        self.convw = dt("convw", [128, 12], F32, kind="ExternalInput").ap()
        self.y = dt("y", [T, D], F32, kind="ExternalOutput").ap()
        self.uvb = dt("uvb", [128 * 128, 2048], BF16, kind="Internal").ap()
        self.x2s = dt("x2s", [T, D], F32, kind="Internal").ap()
        self.idxw_d = dt("idxw_d", [128, 3, T], BF16, kind="Internal").ap()
        if dbg:
            self.dbgt = dt("dbg", list(dbg), F32, kind="ExternalOutput").ap()
        with ExitStack() as es:
            self.es = es
            self.S = Sched(nc, es)
            self.build()

    def sb(self, es, name, shape, dtype):
        return es.enter_context(self.nc.sbuf_tensor(name, list(shape), dtype))

    def build(self):
        nc, S, es = self.nc, self.S, self.es
        T = self.T
        self.ident_bf = self.sb(es, "ident_bf", [128, 128], BF16)
        self.ident_f = self.sb(es, "ident_f", [128, 128], F32)
        self.r_const = S.res("const")
        rc = self.r_const
        S.op("pool", lambda e: e.memset(self.ident_f[:], 0.0), writes=[rc])
        S.op("pool", lambda e: e.affine_select(out=self.ident_f[:], in_=self.ident_f[:], pattern=[[-1, 128]],
                                               compare_op=ALU.not_equal, fill=1.0, base=0, channel_multiplier=1),
             reads=[rc], writes=[rc])
        S.op("pool", lambda e: e.tensor_copy(out=self.ident_bf[:], in_=self.ident_f[:]), reads=[rc], writes=[rc])
        self.banks = [es.enter_context(nc.psum_tensor("bank%d" % i, [128, 512], F32)) for i in range(8)]
        self.rbank = [S.res("bank%d" % i) for i in range(8)]
        self.r_uvb = S.res("uvb")
        self.phase1()
        S.barrier()
        self.phase2()
        S.barrier()

    def convert_uv_piece(self, i):
        S = self.S
        src = (self.uT, self.vC)[i // 16]
        off = (i // 16) * 1024
        j = i % 16
        R = 1024
        S.dma("pool", self.uvb[j * R:(j + 1) * R, off:off + 1024], src[j * R:(j + 1) * R, :], writes=[self.r_uvb], multipart=True)

    def phase1(self):
        nc, S = self.nc, self.S
        T = self.T
        TB = 128
        NB = T // TB
        with ExitStack() as es:
            sb = lambda name, shape, dtype: self.sb(es, name, shape, dtype)
            win = sb("win", [128, 8, INC], BF16)
            pa = sb("pa_sb", [128, 4, D], BF16)
            pb = sb("pb_sb", [128, 4, D], BF16)
            wo = sb("wo_sb", [128, 8, D], BF16)
            r_wA = S.res("w1A")
            r_wB = S.res("w1B")
            r_wC = S.res("w1C")
            r_wp = S.res("w1p")
            r_wo = S.res("w1o")
            for (c0, c1, rr) in ((0, 2048, r_wA), (2048, 3584, r_wB), (3584, INC, r_wC)):
                for k in range(8):
                    S.dma("pool", win[:, k, c0:c1], self.w_in[k * 128:(k + 1) * 128, c0:c1], writes=[rr], multipart=True)
            for k in range(4):
                S.dma("pool", pa[:, k, :], self.pa[k * 128:(k + 1) * 128, :], writes=[r_wp], multipart=True)
                S.dma("pool", pb[:, k, :], self.pb[k * 128:(k + 1) * 128, :], writes=[r_wp], multipart=True)
            for k in range(8):
                S.dma("pool", wo[:, k, :], self.wo[k * 128:(k + 1) * 128, :], writes=[r_wo], multipart=True)

            def r_w_for(c0):
                return r_wA if c0 < 2048 else (r_wB if c0 < 3584 else r_wC)
            g1bc = sb("g1bc_sb", [128, D], F32)
            gobc = sb("gobc_sb", [64, 512], F32)
            lbl = sb("lbl_sb", [64, 16], F32)
            convw = sb("convw_sb", [128, 12], F32)
            r_c = S.res("c1")
            S.dma("sp", g1bc[:], self.g1bc[:, :], writes=[r_c], multipart=True)
            S.dma("sp", gobc[:], self.gobc[:, :], writes=[r_c], multipart=True)
            S.dma("sp", lbl[:], self.lbl[:, :], writes=[r_c], multipart=True)
            S.dma("sp", convw[:], self.convw[:, :], writes=[r_c], multipart=True)
            maskT = sb("maskT", [64, 64], F32)
            m01 = sb("m01", [64, TB], F32)
            r_m = S.res("masks")
            S.op("pool", lambda e: e.memset(maskT[:], 1.0), writes=[r_m])
            S.op("pool", lambda e: e.affine_select(out=maskT[:], in_=maskT[:], pattern=[[1, 64]], compare_op=ALU.is_ge,
                                                   fill=0.0, base=0, channel_multiplier=-1), reads=[r_m], writes=[r_m])
            S.op("pool", lambda e: e.memset(m01[:], 1.0), writes=[r_m])
            S.op("pool", lambda e: e.memset(m01[:, 0:TB:64], 0.0), writes=[r_m])
            lbe = sb("lbe", [64, 16], F32)
            lbs = sb("lbs", [64, 8], F32)
            lb = sb("lb", [64, 8], F32)
            oml = sb("oml", [64, 8], F32)
            r_lb = S.res("lb")
            S.op("act", lambda e: e.activation(out=lbe[:], in_=lbl[:], func=AF.Exp), reads=[r_c], writes=[r_lb])
            S.op("dve", lambda e: e.tensor_tensor(out=lbs[:], in0=lbe[:, 0:8], in1=lbe[:, 8:16], op=ALU.add), reads=[r_lb], writes=[r_lb])
            S.op("dve", lambda e: e.reciprocal(out=lbs[:], in_=lbs[:]), reads=[r_lb], writes=[r_lb])
            S.op("dve", lambda e: e.tensor_tensor(out=lb[:], in0=lbe[:, 0:8], in1=lbs[:], op=ALU.mult), reads=[r_lb], writes=[r_lb])
            S.op("dve", lambda e: e.tensor_scalar(out=oml[:], in0=lb[:], scalar1=-1.0, scalar2=1.0, op0=ALU.mult, op1=ALU.add),
                 reads=[r_lb], writes=[r_lb])
            st = sb("st", [64, 8, 64], F32)
            stb = [sb("stb%d" % i, [64, 8, 64], BF16) for i in range(2)]
            r_st = S.res("st")
            r_stb = [S.res("stb0"), S.res("stb1")]
            S.op("dve", lambda e: e.memset(st[:], 0.0), writes=[r_st])
            S.op("dve", lambda e: e.memset(stb[0][:], 0.0), writes=[r_stb[0]])
            ubuf = sb("ubuf", [128, 4, TB + 2], F32)
            r_u = [S.res("u%d" % j) for j in range(4)]
            S.op("dve", lambda e: e.memset(ubuf[:], 0.0), writes=r_u)
            xin = [sb("xin%d" % i, [128, D], F32) for i in range(2)]
            r_xin = [S.res("xin0"), S.res("xin1")]
            ssq = sb("ssq", [128, 1], F32)
            rstd = sb("rstd", [128, 1], F32)
            r_ssq = S.res("ssq")
            hbs = [sb("hb%d" % i, [128, D], BF16) for i in range(2)]
            r_hbs = [S.res("hb0"), S.res("hb1")]
            hTs = [sb("hT%d" % i, [128, 8, TB], BF16) for i in range(2)]
            r_hTs = [S.res("hT0"), S.res("hT1")]
            thq = sb("thq", [64, 4, TB], BF16)
            sq = sb("sq", [64, 4, TB], BF16)
            fg = sb("fg", [64, 4, TB], F32)
            kk = sb("kk", [64, 4, TB], BF16)
            lf = sb("lf", [64, 4, TB], F32)
            bb = sb("bb", [64, 4, TB], F32)
            r_thq = [S.res("thq%d" % i) for i in range(4)]
            r_sq = [S.res("sq%d" % i) for i in range(4)]
            r_fg = [S.res("fg%d" % i) for i in range(4)]
            r_kk = [S.res("kk%d" % i) for i in range(4)]
            r_lf = [S.res("lf%d" % i) for i in range(4)]
            r_bb = [S.res("bb%d" % i) for i in range(4)]
            nh1 = sb("nh1", [128, 8], F32)
            r_nh1 = S.res("nh1")
            S.op("pool", lambda e: e.memset(nh1[:], -0.5), writes=[r_nh1])
            gobh = sb("gobh", [64, 512], F32)
            r_gobh = S.res("gobh")
            S.op("pool", lambda e: e.tensor_scalar(out=gobh[:], in0=gobc[:], scalar1=0.5, scalar2=None, op0=ALU.mult), reads=[r_c], writes=[r_gobh])
            fc1 = sb("fc1", [64, 8], F32)
            fc0 = sb("fc0", [64, 8], F32)
            S.op("dve", lambda e: e.tensor_scalar(out=fc1[:], in0=oml[:], scalar1=0.5, scalar2=None, op0=ALU.mult), reads=[r_lb], writes=[r_lb])
            S.op("dve", lambda e: e.tensor_tensor(out=fc0[:], in0=lb[:], in1=fc1[:], op=ALU.add), reads=[r_lb], writes=[r_lb])
            qT = sb("qT", [64, 8, TB], BF16)
            kT = sb("kT", [64, 8, TB], BF16)
            r_qT = [S.res("qT%d" % h) for h in range(8)]
            r_kT = [S.res("kT%d" % h) for h in range(8)]
            ebl = sb("ebl", [64, 8, TB // 64], F32)
            r_ebl = [S.res("ebl%d" % h) for h in range(8)]
            v_sb = sb("v_sb", [64, TB // 64, 512], BF16)
            r_v = [S.res("v%d" % c) for c in range(TB // 64)]
            gsg = sb("gsg", [64, TB // 64, 512], F32)
            sgt = sb("sgt", [64, TB // 64, 512], F32)
            r_gsg = [S.res("gsg%d" % c) for c in range(TB // 64)]
            cc_sb = sb("cc_sb", [128, 4, TB], F32)
            cb_sb = sb("cb_sb", [128, 4, TB], F32)
            acc = sb("acc", [128, 4, TB], F32)
            r_cc = [S.res("cc%d" % j) for j in range(4)]
            r_cb = [S.res("cb%d" % j) for j in range(4)]
            r_acc = [S.res("acc%d" % j) for j in range(4)]
            ybT = sb("ybT", [128, 4, TB], BF16)
            r_yb = [S.res("yb%d" % j) for j in range(4)]
            sgT = sb("sgT", [128, 16, TB], BF16)
            r_sg = [S.res("sgT%d" % j) for j in range(16)]
            scm = [sb("scm%d" % i, [64, 8, 64], BF16) for i in range(2)]
            r_scm = [S.res("scm0"), S.res("scm1")]
            ktm = [sb("ktm%d" % i, [64, 8, 64], BF16) for i in range(2)]
            r_ktm = [S.res("ktm0"), S.res("ktm1")]
            sttmp = sb("sttmp", [64, 8, 64], F32)
            r_sttmp = S.res("sttmp")
            sqo = sb("sqo", [64, 8, 64], F32)
            r_sqo = S.res("sqo")
            so = sb("so", [64, 8], F32)
            r_so = S.res("so")
            y1 = sb("y1", [64, 8, 64], F32)
            r_y1 = S.res("y1")
            ya = [sb("ya%d" % i, [64, 512], BF16) for i in range(2)]
            r_ya = [S.res("ya0"), S.res("ya1")]
            yaT = sb("yaT", [128, 4, TB], BF16)
            r_yaT = S.res("yaT")
            mT = sb("mT", [128, 8, TB], BF16)
            r_mT = [S.res("mT%d" % j) for j in range(8)]
            t1 = [sb("t1_%d" % i, [128, TB], F32) for i in range(2)]
            t2 = [sb("t2_%d" % i, [128, TB], F32) for i in range(2)]
            r_t1 = [S.res("t1_0"), S.res("t1_1")]
            r_t2 = [S.res("t2_0"), S.res("t2_1")]
            r_x2d = S.res("x2d")

            bankctr = [0]

            def nb():
                i = bankctr[0] % 8
                bankctr[0] += 1
                return self.banks[i], self.rbank[i]

            LN8 = math.log(0.125)
            NCH = TB // 64
            cctr = [0]

            LNQ = math.log(0.125 * 0.5)
            def head(tb):
                xi, rx = xin[tb % 2], r_xin[tb % 2]
                hb, r_hb = hbs[tb % 2], r_hbs[tb % 2]
                hT, r_hT = hTs[tb % 2], r_hTs[tb % 2]
                S.op("act", lambda e: e.activation(out=hb[:], in_=xi[:], func=AF.Square, accum_out=ssq[:]),
                     reads=[rx], writes=[r_hb, r_ssq])
                S.op("pool", lambda e: e.tensor_scalar(out=rstd[:], in0=ssq[:], scalar1=1.0 / D, scalar2=EPS, op0=ALU.mult, op1=ALU.add),
                     reads=[r_ssq], writes=[r_ssq])
                S.op("pool", lambda e: e.tensor_tensor(out=rstd[:], in0=rstd[:], in1=nh1[:, 0:1], op=ALU.pow), reads=[r_ssq, r_nh1], writes=[r_ssq])
                S.op("dve", lambda e: e.scalar_tensor_tensor(out=hb[:], in0=xi[:], scalar=rstd[:], in1=g1bc[:], op0=ALU.mult, op1=ALU.mult),
                     reads=[rx, r_ssq, r_c], writes=[r_hb])
                bk, rb = nb()
                bkb = bk[:].bitcast(BF16)
                for k in range(8):
                    S.op("pe", lambda e, k=k: e.transpose(out=bkb[:, k * 128:(k + 1) * 128], in_=hb[:, k * 128:(k + 1) * 128], identity=self.ident_bf[:]),
                         reads=[r_hb, self.r_const], writes=[rb])
                S.op("act", lambda e: e.activation(out=hT[:], in_=bkb.rearrange("p (k t) -> p k t", k=8), func=AF.Copy),
                     reads=[rb], writes=[r_hT])

            S.dma("sp", xin[0][:], self.x[0:TB, :], writes=[r_xin[0]])
            if NB > 1:
                S.dma("sp", xin[1][:], self.x[TB:2 * TB, :], writes=[r_xin[1]])
            head(0)
            for tb in range(NB):
                xi = xin[tb % 2]
                rx = r_xin[tb % 2]
                hT, r_hT = hTs[tb % 2], r_hTs[tb % 2]

                def proj_fm(c0, ncols, bk, rb):
                    for k in range(8):
                        S.op("pe", lambda e, k=k: e.matmul(bk[0:ncols, 0:TB], lhsT=win[:, k, c0:c0 + ncols], rhs=hT[:, k, :],
                                                            start=(k == 0), stop=(k == 7)),
                             reads=[r_hT, r_w_for(c0)], writes=[rb])

                for g in range(2):
                    for hi in range(4):
                        h = 4 * g + hi
                        bk, rb = nb()
                        proj_fm(h * 64, 64, bk, rb)
                        S.op("act", lambda e: e.activation(out=thq[:, hi, :], in_=bk[0:64, 0:TB], func=AF.Tanh, scale=0.5), reads=[rb], writes=[r_thq[hi]])
                        S.op("dve", lambda e: e.scalar_tensor_tensor(out=sq[:, hi, :], in0=thq[:, hi, :], scalar=1.0, in1=bk[0:64, 0:TB], op0=ALU.add, op1=ALU.mult),
                             reads=[r_thq[hi], rb], writes=[r_sq[hi]])
                    if g == 0:
                        for c in range(NCH):
                            bk, rb = nb()
                            for k in range(8):
                                S.op("pe", lambda e, k=k: e.matmul(bk[0:64, :], lhsT=hT[:, k, c * 64:(c + 1) * 64], rhs=win[:, k, 1536:2048],
                                                                    start=(k == 0), stop=(k == 7)), reads=[r_hT, r_wA], writes=[rb])
                            S.op("act", lambda e: e.activation(out=sgt[:, c, :], in_=bk[0:64, :], func=AF.Tanh, scale=0.5), reads=[rb], writes=[r_gsg[c]])
                            S.op("dve", lambda e: e.scalar_tensor_tensor(out=gsg[:, c, :], in0=sgt[:, c, :], scalar=1.0, in1=bk[0:64, :], op0=ALU.add, op1=ALU.mult),
                                 reads=[r_gsg[c], rb], writes=[r_gsg[c]])
                            S.op("dve", lambda e: e.tensor_tensor(out=gsg[:, c, :], in0=gsg[:, c, :], in1=gobh[:], op=ALU.mult),
                                 reads=[r_gsg[c], r_gobh], writes=[r_gsg[c]])
                    for hi in range(4):
                        h = 4 * g + hi
                        bk, rb = nb()
                        proj_fm(512 + h * 64, 64, bk, rb)
                        S.op("act", lambda e: e.activation(out=fg[:, hi, :], in_=bk[0:64, 0:TB], func=AF.Tanh, scale=0.5), reads=[rb], writes=[r_fg[hi]])
                        S.op("dve", lambda e: e.tensor_scalar(out=fg[:, hi, :], in0=fg[:, hi, :], scalar1=fc1[:, h:h + 1], scalar2=fc0[:, h:h + 1],
                                                              op0=ALU.mult, op1=ALU.add), reads=[r_fg[hi], r_lb], writes=[r_fg[hi]])
                        S.op("dve", lambda e: e.tensor_scalar(out=kk[:, hi, :], in0=fg[:, hi, :], scalar1=-1.0, scalar2=1.0, op0=ALU.mult, op1=ALU.add),
                             reads=[r_fg[hi]], writes=[r_kk[hi]])
                    if g == 0:
                        for c in range(NCH):
                            bk, rb = nb()
                            for k in range(8):
                                S.op("pe", lambda e, k=k: e.matmul(bk[0:64, :], lhsT=hT[:, k, c * 64:(c + 1) * 64], rhs=win[:, k, 1024:1536],
                                                                    start=(k == 0), stop=(k == 7)), reads=[r_hT, r_wA], writes=[rb])
                            S.op("act", lambda e: e.activation(out=v_sb[:, c, :], in_=bk[0:64, :], func=AF.Copy), reads=[rb], writes=[r_v[c]])
                    for hi in range(4):
                        S.op("act", lambda e: e.activation(out=lf[:, hi, :], in_=fg[:, hi, :], func=AF.Ln), reads=[r_fg[hi]], writes=[r_lf[hi]])
                    for hi in range(4):
                        S.op("dve", lambda e: e.tensor_tensor_scan(out=bb[:, hi, :], data0=m01[:], data1=lf[:, hi, :], initial=0.0, op0=ALU.mult, op1=ALU.add),
                             reads=[r_lf[hi], r_m], writes=[r_bb[hi]])
                    for hi in range(4):
                        h = 4 * g + hi
                        S.op("act", lambda e: e.activation(out=ebl[:, h, :], in_=bb[:, hi, 63:TB:64], func=AF.Exp), reads=[r_bb[hi]], writes=[r_ebl[h]])
                        S.op("act", lambda e: e.activation(out=lf[:, hi, :], in_=bb[:, hi, :], func=AF.Exp, bias=LNQ), reads=[r_bb[hi], r_lf[hi]], writes=[r_lf[hi]])
                        S.op("act", lambda e: e.activation(out=bb[:, hi, :], in_=bb[:, hi, :], func=AF.Exp, scale=-1.0), reads=[r_bb[hi]], writes=[r_bb[hi]])
                    for hi in range(4):
                        h = 4 * g + hi
                        S.op("dve", lambda e: e.tensor_tensor(out=qT[:, h, :], in0=sq[:, hi, :], in1=lf[:, hi, :], op=ALU.mult), reads=[r_sq[hi], r_lf[hi]], writes=[r_qT[h]])
                        S.op("dve", lambda e: e.tensor_tensor(out=kT[:, h, :], in0=kk[:, hi, :], in1=bb[:, hi, :], op=ALU.mult), reads=[r_kk[hi], r_bb[hi]], writes=[r_kT[h]])

                fillq = []

                def conv_group(j, which):
                    def f():
                        bk, rb = nb()
                        if which == 0:
                            proj_fm(2560 + j * 128, 128, bk, rb)
                            S.op("act", lambda e: e.activation(out=cc_sb[:, j, :], in_=bk[:, 0:TB], func=AF.Copy), reads=[rb], writes=[r_cc[j]])
                        elif which == 1:
                            proj_fm(3072 + j * 128, 128, bk, rb)
                            S.op("dve", lambda e: e.tensor_tensor(out=ubuf[:, j, 2:TB + 2], in0=bk[:, 0:TB], in1=cc_sb[:, j, :], op=ALU.mult),
                                 reads=[rb, r_cc[j]], writes=[r_u[j]])
                        else:
                            proj_fm(2048 + j * 128, 128, bk, rb)
                            S.op("act", lambda e: e.activation(out=cb_sb[:, j, :], in_=bk[:, 0:TB], func=AF.Copy), reads=[rb], writes=[r_cb[j]])
                            S.op("dve", lambda e: e.tensor_scalar(out=acc[:, j, :], in0=ubuf[:, j, 2:TB + 2], scalar1=convw[:, j * 3 + 2:j * 3 + 3],
                                                                  scalar2=None, op0=ALU.mult), reads=[r_u[j], r_c], writes=[r_acc[j]])
                            S.op("dve", lambda e: e.scalar_tensor_tensor(out=acc[:, j, :], in0=ubuf[:, j, 1:TB + 1], scalar=convw[:, j * 3 + 1:j * 3 + 2],
                                                                         in1=acc[:, j, :], op0=ALU.mult, op1=ALU.add),
                                 reads=[r_u[j], r_c, r_acc[j]], writes=[r_acc[j]])
                            S.op("dve", lambda e: e.scalar_tensor_tensor(out=acc[:, j, :], in0=ubuf[:, j, 0:TB], scalar=convw[:, j * 3:j * 3 + 1],
                                                                         in1=acc[:, j, :], op0=ALU.mult, op1=ALU.add),
                                 reads=[r_u[j], r_c, r_acc[j]], writes=[r_acc[j]])
                            S.op("dve", lambda e: e.tensor_tensor(out=ybT[:, j, :], in0=acc[:, j, :], in1=cb_sb[:, j, :], op=ALU.mult),
                                 reads=[r_acc[j], r_cb[j]], writes=[r_yb[j]])
                            S.op("dve", lambda e: e.tensor_copy(out=ubuf[:, j, 0:2], in_=ubuf[:, j, TB:TB + 2]), reads=[r_u[j]], writes=[r_u[j]])
                    return f

                def gate_group(j):
                    def f():
                        bk, rb = nb()
                        proj_fm(3584 + j * 128, 128, bk, rb)
                        S.op("act", lambda e: e.activation(out=sgT[:, j, :], in_=bk[:, 0:TB], func=AF.Tanh, scale=0.5), reads=[rb], writes=[r_sg[j]])
                    return f

                for j in range(4):
                    for which in range(3):
                        fillq.append(conv_group(j, which))
                for j in range(16):
                    fillq.append(gate_group(j))

                def fill(n):
                    for _ in range(n):
                        if fillq:
                            fillq.pop(0)()

                fill(4)
                for c in range(NCH):
                    g = cctr[0]
                    cctr[0] += 1
                    cs = slice(c * 64, (c + 1) * 64)
                    sc, rsc = scm[g % 2], r_scm[g % 2]
                    kt, rkt = ktm[g % 2], r_ktm[g % 2]
                    sb_cur, rsb_cur = stb[g % 2], r_stb[g % 2]
                    sb_nxt, rsb_nxt = stb[(g + 1) % 2], r_stb[(g + 1) % 2]
                    bkA, rbA = nb()
                    for h in range(8):
                        S.op("pe", lambda e, h=h: e.matmul(bkA[0:64, h * 64:(h + 1) * 64], lhsT=kT[:, h, cs], rhs=qT[:, h, cs], start=True, stop=True),
                             reads=[r_kT[h], r_qT[h]], writes=[rbA])
                    bkB, rbB = nb()
                    bkBb = bkB[:].bitcast(BF16)
                    for h in range(8):
                        S.op("pe", lambda e, h=h: e.transpose(out=bkBb[0:64, h * 64:(h + 1) * 64], in_=kT[:, h, cs], identity=self.ident_bf[0:64, 0:64]),
                             reads=[r_kT[h], self.r_const], writes=[rbB])
                    S.op("dve", lambda e: e.tensor_tensor(out=sc[:], in0=bkA[0:64, :].rearrange("p (h t) -> p h t", h=8),
                                                          in1=maskT[:].unsqueeze(1).broadcast_to([64, 8, 64]), op=ALU.mult),
                         reads=[rbA, r_m], writes=[rsc])
                    S.op("act", lambda e: e.activation(out=kt[:], in_=bkBb[0:64, 0:512].rearrange("p (h d) -> p h d", h=8), func=AF.Copy),
                         reads=[rbB], writes=[rkt])
                    fill(3)
                    bkC, rbC = nb()
                    for h in range(8):
                        S.op("pe", lambda e, h=h: e.matmul(bkC[0:64, h * 64:(h + 1) * 64], lhsT=kt[:, h, :], rhs=v_sb[:, c, h * 64:(h + 1) * 64], start=True, stop=True),
                             reads=[rkt, r_v[c]], writes=[rbC])
                    bkD, rbD = nb()
                    for h in range(8):
                        S.op("pe", lambda e, h=h: e.matmul(bkD[0:64, h * 64:(h + 1) * 64], lhsT=sc[:, h, :], rhs=v_sb[:, c, h * 64:(h + 1) * 64], start=True, stop=False),
                             reads=[rsc, r_v[c]], writes=[rbD])
                        S.op("pe", lambda e, h=h: e.matmul(bkD[0:64, h * 64:(h + 1) * 64], lhsT=qT[:, h, cs], rhs=sb_cur[:, h, :], start=False, stop=True),
                             reads=[r_qT[h], rsb_cur], writes=[rbD])
                    S.op("dve", lambda e: e.tensor_tensor(out=sttmp[:], in0=bkC[0:64, :].rearrange("p (h e) -> p h e", h=8), in1=st[:], op=ALU.add),
                         reads=[rbC, r_st], writes=[r_sttmp])
                    S.op("dve", lambda e: e.tensor_tensor(out=st[:], in0=sttmp[:], in1=ebl[:, :, c:c + 1].broadcast_to([64, 8, 64]), op=ALU.mult),
                         reads=[r_sttmp] + r_ebl, writes=[r_st])
                    S.op("dve", lambda e: e.tensor_copy(out=sb_nxt[:], in_=st[:]), reads=[r_st], writes=[rsb_nxt])
                    S.op("act", lambda e: e.activation(out=sqo[:], in_=bkD[0:64, :].rearrange("p (h e) -> p h e", h=8), func=AF.Square),
                         reads=[rbD], writes=[r_sqo])
                    fill(3)
                    S.op("dve", lambda e: e.tensor_reduce(out=so[:], in_=sqo[:], axis=AX.X, op=ALU.add), reads=[r_sqo], writes=[r_so])
                    S.op("pool", lambda e: e.tensor_scalar(out=so[:], in0=so[:], scalar1=1.0 / 64, scalar2=EPS, op0=ALU.mult, op1=ALU.add),
                         reads=[r_so], writes=[r_so])
                    S.op("pool", lambda e: e.tensor_tensor(out=so[:], in0=so[:], in1=nh1[0:64, :], op=ALU.pow), reads=[r_so, r_nh1], writes=[r_so])
                    fill(3)
                    S.op("dve", lambda e: e.tensor_tensor(out=y1[:], in0=bkD[0:64, :].rearrange("p (h e) -> p h e", h=8),
                                                          in1=so[:].unsqueeze(2).broadcast_to([64, 8, 64]), op=ALU.mult),
                         reads=[rbD, r_so], writes=[r_y1])
                    yac, ryac = ya[g % 2], r_ya[g % 2]
                    S.op("dve", lambda e: e.tensor_tensor(out=yac[:], in0=y1[:].rearrange("p h e -> p (h e)"), in1=gsg[:, c, :], op=ALU.mult),
                         reads=[r_y1, r_gsg[c]], writes=[ryac])
                    fill(3)
                    bkE, rbE = nb()
                    bkEb = bkE[:].bitcast(BF16)
                    for k in range(4):
                        S.op("pe", lambda e, k=k: e.transpose(out=bkEb[:, k * 64:(k + 1) * 64], in_=yac[:, k * 128:(k + 1) * 128], identity=self.ident_bf[0:64, 0:64]),
                             reads=[ryac, self.r_const], writes=[rbE])
                    S.op("act", lambda e: e.activation(out=yaT[:, :, cs], in_=bkEb[:, 0:256].rearrange("p (k t) -> p k t", k=4), func=AF.Copy),
                         reads=[rbE], writes=[r_yaT])
                fill(100)
                npiece = (32 + NB - 1) // NB
                for pi in range(tb * npiece, min(32, (tb + 1) * npiece)):
                    self.convert_uv_piece(pi)
                if tb + 1 < NB:
                    head(tb + 1)
                for j in range(8):
                    bkA, rbA = nb()
                    for k in range(4):
                        S.op("pe", lambda e, k=k: e.matmul(bkA[:, 0:TB], lhsT=pa[:, k, j * 128:(j + 1) * 128], rhs=yaT[:, k, :], start=(k == 0), stop=(k == 3)),
                             reads=[r_yaT, r_wp], writes=[rbA])
                    bkB, rbB = nb()
                    for k in range(4):
                        S.op("pe", lambda e, k=k: e.matmul(bkB[:, 0:TB], lhsT=pb[:, k, j * 128:(j + 1) * 128], rhs=ybT[:, k, :], start=(k == 0), stop=(k == 3)),
                             reads=r_yb + [r_wp], writes=[rbB])
                    a1, ra1 = t1[j % 2], r_t1[j % 2]
                    a2, ra2 = t2[j % 2], r_t2[j % 2]
                    S.op("dve", lambda e: e.scalar_tensor_tensor(out=a1[:], in0=sgT[:, j, :], scalar=1.0, in1=bkA[:, 0:TB], op0=ALU.add, op1=ALU.mult),
                         reads=[rbA, r_sg[j]], writes=[ra1])
                    S.op("dve", lambda e: e.scalar_tensor_tensor(out=a2[:], in0=sgT[:, 8 + j, :], scalar=1.0, in1=bkB[:, 0:TB], op0=ALU.add, op1=ALU.mult),
                         reads=[rbB, r_sg[8 + j]], writes=[ra2])
                    S.op("dve", lambda e: e.tensor_tensor(out=mT[:, j, :], in0=a1[:], in1=a2[:], op=ALU.add), reads=[ra1, ra2], writes=[r_mT[j]])
                for half in range(2):
                    bk, rb = nb()
                    for k in range(8):
                        S.op("pe", lambda e, k=k: e.matmul(bk[:, :], lhsT=mT[:, k, :], rhs=wo[:, k, half * 512:(half + 1) * 512], start=(k == 0), stop=(k == 7)),
                             reads=[r_mT[k], r_wo], writes=[rb])
                    S.op("dve", lambda e: e.scalar_tensor_tensor(out=xi[:, half * 512:(half + 1) * 512], in0=bk[:, :], scalar=0.5,
                                                                 in1=xi[:, half * 512:(half + 1) * 512], op0=ALU.mult, op1=ALU.add),
                         reads=[rb, rx], writes=[rx])
                S.dma("sp", self.x2s[tb * TB:(tb + 1) * TB, :], xi[:], reads=[rx], writes=[r_x2d], semres=rx)
                if tb + 2 < NB:
                    S.dma("sp", xi[:], self.x[(tb + 2) * TB:(tb + 3) * TB, :], writes=[rx])

    def phase2(self):
        nc, S = self.nc, self.S
        T = self.T
        TB = 256
        NB = T // TB
        idxw_d = self.idxw_d
        r_idxd = [S.res("idxd%d" % i) for i in range(T // 128)]
        with ExitStack() as es0:
            sb0 = lambda name, shape, dtype: self.sb(es0, name, shape, dtype)
            g2bc = sb0("g2bc_sb", [128, D], F32)
            gfbc = sb0("gfbc_sb", [128, D], F32)
            r_c = S.res("c2")
            S.dma("sp", g2bc[:], self.g2bc[:, :], writes=[r_c], multipart=True)
            S.dma("sp", gfbc[:], self.gfbc[:, :], writes=[r_c], multipart=True)
            iota_f = sb0("iota_f", [128, 128], F32)
            iota_bf = sb0("iota_bf", [128, 128], BF16)
            iotaKA = sb0("iotaKA", [128, 16, 16], F32)
            r_io = S.res("iota")
            S.op("pool", lambda e: e.iota(iota_f[:], pattern=[[1, 128]], base=0, channel_multiplier=0, allow_small_or_imprecise_dtypes=True), writes=[r_io])
            S.op("pool", lambda e: e.tensor_copy(out=iota_bf[:], in_=iota_f[:]), reads=[r_io], writes=[r_io])
            S.op("pool", lambda e: e.iota(iotaKA[:], pattern=[[0, 16], [1, 16]], base=0, channel_multiplier=0, allow_small_or_imprecise_dtypes=True), writes=[r_io])
            xs = sb0("xs", [128, D], F32)
            r_xs = S.res("xs")
            junk = sb0("junk2", [128, D], BF16)
            r_junk = S.res("junk2")
            ssq = sb0("ssq2", [128, 2], F32)
            rstd = sb0("rstd2", [128, 2], F32)
            r_ssq = S.res("ssq2")
            h2 = sb0("h2", [128, D], BF16)
            r_h2 = S.res("h2")
            h2T = [sb0("h2T%d" % i, [128, 8, TB], BF16) for i in range(2)]
            r_h2T = [S.res("h2T0"), S.res("h2T1")]

            def load_norm_tile(b, tt, bank_idx):
                hT, rhT = h2T[b % 2], r_h2T[b % 2]
                r0 = b * TB + tt * 128
                S.dma("pool", xs[:], self.x2s[r0:r0 + 128, :], writes=[r_xs])
                S.op("act", lambda e: e.activation(out=junk[:], in_=xs[:], func=AF.Square, accum_out=ssq[:, 0:1]),
                     reads=[r_xs], writes=[r_junk, r_ssq])
                S.op("dve", lambda e: e.tensor_scalar(out=rstd[:, 0:1], in0=ssq[:, 0:1], scalar1=1.0 / D, scalar2=EPS, op0=ALU.mult, op1=ALU.add),
                     reads=[r_ssq], writes=[r_ssq])
                S.op("act", lambda e: e.activation(out=rstd[:, 0:1], in_=rstd[:, 0:1], func=AF.Sqrt), reads=[r_ssq], writes=[r_ssq])
                S.op("dve", lambda e: e.reciprocal(out=rstd[:, 0:1], in_=rstd[:, 0:1]), reads=[r_ssq], writes=[r_ssq])
                S.op("dve", lambda e: e.scalar_tensor_tensor(out=h2[:], in0=xs[:], scalar=rstd[:, 0:1], in1=g2bc[:],
                                                             op0=ALU.mult, op1=ALU.mult), reads=[r_xs, r_ssq, r_c], writes=[r_h2])
                bk, rb = self.banks[bank_idx], self.rbank[bank_idx]
                bkb = bk[:].bitcast(BF16)
                for k in range(8):
                    S.op("pe", lambda e, k=k: e.transpose(out=bkb[:, k * 128:(k + 1) * 128], in_=h2[:, k * 128:(k + 1) * 128], identity=self.ident_bf[:]),
                         reads=[r_h2, self.r_const], writes=[rb])
                S.op("act", lambda e: e.activation(out=hT[:, :, tt * 128:(tt + 1) * 128], in_=bkb.rearrange("p (k t) -> p k t", k=8), func=AF.Copy),
                     reads=[rb], writes=[rhT])

            with ExitStack() as es:
                sb = lambda name, shape, dtype: self.sb(es, name, shape, dtype)
                wq = sb("wq_sb", [128, 8, 2048], BF16)
                keysT = sb("keysT_sb", [128, 16, 128], BF16)
                r_w = S.res("w2")
                for k in range(8):
                    S.dma("pool", wq[:, k, :], self.wq[k * 128:(k + 1) * 128, :], writes=[r_w], multipart=True)
                S.dma("pool", keysT[:].rearrange("p g n -> p (g n)"), self.keysT[:, :], writes=[r_w], multipart=True)
                qT = [sb("qT2_%d" % i, [128, 16, TB], BF16) for i in range(2)]
                r_qT = [[S.res("qT2_%d_%d" % (i, g)) for g in range(16)] for i in range(2)]
                s_sb = [sb("s_sb%d" % i, [128, 16, 128], F32) for i in range(4)]
                r_s = [[S.res("s%d_%d" % (i, g)) for g in range(16)] for i in range(4)]

                class KS:
                    pass

                def mkset(p):
                    k = KS()
                    n = lambda s: "%s_p%d" % (s, p)
                    k.s_tmp = sb(n("s_tmp"), [128, 16, 128], F32)
                    k.r_stmp = [S.res(n("stmp%d" % g)) for g in range(16)]
                    k.eqs = sb(n("eqs"), [128, 8, 16, 16], F32)
                    k.r_eq = S.res(n("eqs"))
                    k.v16 = sb(n("v16"), [128, 16, 16], F32)
                    k.i16 = sb(n("i16"), [128, 16, 16], U32)
                    k.r_v16 = [S.res(n("v16_%d" % g)) for g in range(16)]
                    k.r_i16 = [S.res(n("i16_%d" % g)) for g in range(16)]
                    k.i16f = sb(n("i16f"), [128, 16, 16], F32)
                    k.r_i16f = S.res(n("i16f"))
                    k.cand = sb(n("cand"), [128, 8, 256], F32)
                    k.r_cand = [S.res(n("cand%d" % h)) for h in range(8)]
                    k.ts16 = sb(n("ts16"), [128, 8, 16], F32)
                    k.pos = sb(n("pos"), [128, 8, 16], U32)
                    k.r_ts = [S.res(n("ts%d" % h)) for h in range(8)]
                    k.r_pos = [S.res(n("pos%d" % h)) for h in range(8)]
                    k.au = sb(n("au"), [128, 8, 16], U32)
                    k.bu = sb(n("bu"), [128, 8, 16], U32)
                    k.af = sb(n("af"), [128, 8, 16], F32)
                    k.bf = sb(n("bf"), [128, 8, 16], F32)
                    k.r_ab = S.res(n("ab"))
                    k.sel = sb(n("sel"), [128, 3, 128], F32)
                    k.r_sel = S.res(n("sel"))
                    k.ew = sb(n("ew"), [128, 8, 16], F32)
                    k.zz = sb(n("zz"), [128, 8], F32)
                    k.r_ew = S.res(n("ew"))
                    k.stg = sb(n("stg"), [128, 3, 128], BF16)
                    k.r_stg = S.res(n("stg"))
                    return k

                ksets = [mkset(0), mkset(1)]
                bctr = [0]

                def nb():
                    i = bctr[0] % 8
                    bctr[0] += 1
                    return self.banks[i], self.rbank[i]

                def stage_Q(b):
                    for tt in range(2):
                        i = bctr[0] % 8
                        bctr[0] += 1
                        load_norm_tile(b, tt, i)
                    hT, rhT = h2T[b % 2], r_h2T[b % 2]
                    for qc in range(16):
                        bk, rb = nb()
                        for k in range(8):
                            S.op("pe", lambda e, k=k: e.matmul(bk[:, 0:TB], lhsT=wq[:, k, qc * 128:(qc + 1) * 128], rhs=hT[:, k, :],
                                                                start=(k == 0), stop=(k == 7)), reads=[rhT, r_w], writes=[rb])
                        S.op("act", lambda e: e.activation(out=qT[b % 2][:, qc, :], in_=bk[:, 0:TB], func=AF.Copy), reads=[rb], writes=[r_qT[b % 2][qc]])

                def stage_S(b, tt):
                    si = (b % 2) * 2 + tt
                    tsl = slice(tt * 128, (tt + 1) * 128)
                    ss, rs = s_sb[si], r_s[si]
                    for g0 in range(0, 16, 4):
                        bk, rb = nb()
                        for gi in range(4):
                            g = g0 + gi
                            j, h = g // 8, g % 8
                            S.op("pe", lambda e: e.matmul(bk[:, gi * 128:(gi + 1) * 128], lhsT=qT[b % 2][:, h * 2 + j, tsl], rhs=keysT[:, g, :],
                                                          start=True, stop=True), reads=[r_qT[b % 2][h * 2 + j], r_w], writes=[rb])
                        S.op("act", lambda e: e.activation(out=ss[:, g0:g0 + 4, :], in_=bk[:].rearrange("p (g n) -> p g n", g=4), func=AF.Copy),
                             reads=[rb], writes=rs[g0:g0 + 4])

                def stage_K(b, tt):
                    n = b * 2 + tt
                    si = (b % 2) * 2 + tt
                    ss, rs = s_sb[si], r_s[si]
                    K = ksets[tt]
                    v16, i16, s_tmp, cand, ts16, pos = K.v16, K.i16, K.s_tmp, K.cand, K.ts16, K.pos
                    for g in range(16):
                        S.op("dve", lambda e: e.max(out=v16[:, g, 0:8], in_=ss[:, g, :]), reads=[rs[g]], writes=[K.r_v16[g]])
                    yield
                    for g in range(16):
                        S.op("dve", lambda e: e.max_index(out=i16[:, g, 0:8], in_max=v16[:, g, 0:8], in_values=ss[:, g, :]),
                             reads=[rs[g], K.r_v16[g]], writes=[K.r_i16[g]])
                    yield
                    for g in range(16):
                        S.op("dve", lambda e: e.match_replace(out=s_tmp[:, g, :], in_to_replace=v16[:, g, 0:8], in_values=ss[:, g, :], imm_value=NEG),
                             reads=[rs[g], K.r_v16[g]], writes=[K.r_stmp[g]])
                    yield
                    for g in range(16):
                        S.op("dve", lambda e: e.max(out=v16[:, g, 8:16], in_=s_tmp[:, g, :]), reads=[K.r_stmp[g]], writes=[K.r_v16[g]])
                    yield
                    for g in range(16):
                        S.op("dve", lambda e: e.max_index(out=i16[:, g, 8:16], in_max=v16[:, g, 8:16], in_values=s_tmp[:, g, :]),
                             reads=[K.r_stmp[g], K.r_v16[g]], writes=[K.r_i16[g]])
                    yield
                    S.op("act", lambda e: e.activation(out=K.i16f[:], in_=i16[:], func=AF.Copy), reads=K.r_i16, writes=[K.r_i16f])
                    S.op("pool", lambda e: e.tensor_tensor(out=cand[:].rearrange("p h (a b) -> p h a b", a=16),
                                                          in0=v16[:, 0:8, :].unsqueeze(3).broadcast_to([128, 8, 16, 16]),
                                                          in1=v16[:, 8:16, :].unsqueeze(2).broadcast_to([128, 8, 16, 16]), op=ALU.add),
                         reads=K.r_v16, writes=K.r_cand)
                    yield
                    ctmp = s_tmp[:].rearrange("p g n -> p (g n)").rearrange("p (h c) -> p h c", h=8)
                    for h in range(8):
                        S.op("dve", lambda e: e.max(out=ts16[:, h, 0:8], in_=cand[:, h, :]), reads=[K.r_cand[h]], writes=[K.r_ts[h]])
                    yield
                    for h in range(8):
                        S.op("dve", lambda e: e.max_index(out=pos[:, h, 0:8], in_max=ts16[:, h, 0:8], in_values=cand[:, h, :]),
                             reads=[K.r_cand[h], K.r_ts[h]], writes=[K.r_pos[h]])
                    yield
                    for h in range(8):
                        S.op("dve", lambda e: e.match_replace(out=ctmp[:, h, :], in_to_replace=ts16[:, h, 0:8], in_values=cand[:, h, :], imm_value=NEG),
                             reads=[K.r_cand[h], K.r_ts[h]], writes=K.r_stmp[2 * h:2 * h + 2])
                    yield
                    for h in range(8):
                        S.op("dve", lambda e: e.max(out=ts16[:, h, 8:16], in_=ctmp[:, h, :]), reads=K.r_stmp[2 * h:2 * h + 2], writes=[K.r_ts[h]])
                    yield
                    for h in range(8):
                        S.op("dve", lambda e: e.max_index(out=pos[:, h, 8:16], in_max=ts16[:, h, 8:16], in_values=ctmp[:, h, :]),
                             reads=K.r_stmp[2 * h:2 * h + 2] + [K.r_ts[h]], writes=[K.r_pos[h]])
                    yield
                    S.op("dve", lambda e: e.tensor_scalar(out=K.au[:], in0=pos[:], scalar1=4, scalar2=None, op0=ALU.logical_shift_right),
                         reads=K.r_pos, writes=[K.r_ab])
                    S.op("dve", lambda e: e.tensor_scalar(out=K.bu[:], in0=pos[:], scalar1=15, scalar2=None, op0=ALU.bitwise_and),
                         reads=K.r_pos, writes=[K.r_ab])
                    S.op("pool", lambda e: e.tensor_tensor(out=K.ew[:], in0=ts16[:], in1=ts16[:, :, 0:1].broadcast_to([128, 8, 16]), op=ALU.subtract),
                         reads=K.r_ts, writes=[K.r_ew])
                    S.op("act", lambda e: e.activation(out=K.ew[:], in_=K.ew[:], func=AF.Exp), reads=[K.r_ew], writes=[K.r_ew])
                    yield
                    S.op("act", lambda e: e.activation(out=K.af[:], in_=K.au[:], func=AF.Copy), reads=[K.r_ab], writes=[K.r_ab])
                    S.op("act", lambda e: e.activation(out=K.bf[:], in_=K.bu[:], func=AF.Copy), reads=[K.r_ab], writes=[K.r_ab])
                    yield
                    eq = K.eqs[:]
                    for which, sel_ab, joff in ((0, K.af, 0), (1, K.bf, 8)):
                        S.op("dve", lambda e: e.tensor_tensor(out=eq, in0=sel_ab[:].unsqueeze(3).broadcast_to([128, 8, 16, 16]),
                                                              in1=iotaKA[:].unsqueeze(1).broadcast_to([128, 8, 16, 16]), op=ALU.is_equal),
                             reads=[K.r_ab, r_io], writes=[K.r_eq])
                        yield
                        S.op("dve", lambda e: e.tensor_tensor(out=eq, in0=eq, in1=K.i16f[:, joff:joff + 8, :].unsqueeze(2).broadcast_to([128, 8, 16, 16]),
                                                              op=ALU.mult), reads=[K.r_eq, K.r_i16f], writes=[K.r_eq])
                        yield
                        S.op("dve", lambda e: e.tensor_reduce(out=K.sel[:, which, :].rearrange("p (h k) -> p h k", h=8), in_=eq, axis=AX.X, op=ALU.add),
                             reads=[K.r_eq], writes=[K.r_sel])
                        yield
                    S.op("dve", lambda e: e.tensor_reduce(out=K.zz[:], in_=K.ew[:], axis=AX.X, op=ALU.add), reads=[K.r_ew], writes=[K.r_ew])
                    yield
                    S.op("dve", lambda e: e.reciprocal(out=K.zz[:], in_=K.zz[:]), reads=[K.r_ew], writes=[K.r_ew])
                    yield
                    S.op("pool", lambda e: e.tensor_tensor(out=K.sel[:, 2, :].rearrange("p (h k) -> p h k", h=8), in0=K.ew[:],
                                                          in1=K.zz[:].unsqueeze(2).broadcast_to([128, 8, 16]), op=ALU.mult),
                         reads=[K.r_ew], writes=[K.r_sel])
                    yield
                    bk, rb = nb()
                    for i in range(3):
                        S.op("pe", lambda e, i=i: e.transpose(out=bk[:, i * 128:(i + 1) * 128], in_=K.sel[:, i, :], identity=self.ident_f[:]),
                             reads=[K.r_sel, self.r_const], writes=[rb])
                    yield
                    S.op("act", lambda e: e.activation(out=K.stg[:], in_=bk[:, 0:384].rearrange("p (i t) -> p i t", i=3), func=AF.Copy),
                         reads=[rb], writes=[K.r_stg])
                    S.dma("sp", idxw_d[:, :, n * 128:(n + 1) * 128], K.stg[:], reads=[K.r_stg], writes=[r_idxd[n]], semres=K.r_stg)

                def interleave(gens):
                    gens = list(gens)
                    while gens:
                        for g in list(gens):
                            try:
                                next(g)
                            except StopIteration:
                                gens.remove(g)

                stage_Q(0)
                stage_S(0, 0)
                stage_S(0, 1)
                for b in range(NB):
                    if b + 1 < NB:
                        stage_Q(b + 1)
                        stage_S(b + 1, 0)
                        stage_S(b + 1, 1)
                    interleave([stage_K(b, 0), stage_K(b, 1)])
            S.barrier()
            with ExitStack() as es:
                sb = lambda name, shape, dtype: self.sb(es, name, shape, dtype)
                Gs = [sb("Gs%d" % i, [128, TB, 128], BF16) for i in range(2)]
                r_Gs = [[S.res("Gs%d_%d" % (i, j)) for j in range(TB // 4)] for i in range(2)]
                NU = 8
                uvslot = [sb("uvslot%d" % i, [128, 2048], BF16) for i in range(NU)]
                r_uv = [S.res("uv%d" % i) for i in range(NU)]
                idxb = [sb("idxb%d" % i, [128, 3, TB], BF16) for i in range(2)]
                r_idxb = [S.res("idxb0"), S.res("idxb1")]
                ga = [sb("ga%d" % i, [128, TB], BF16) for i in range(2)]
                gaT = [sb("gaT%d" % i, [128, TB], BF16) for i in range(2)]
                r_ga = [S.res("ga0"), S.res("ga1")]
                r_gaT = [S.res("gaT0"), S.res("gaT1")]
                x2b = sb("x2b", [128, 2, D], F32)
                r_x2b = S.res("x2b")
                r_yd = S.res("yd")
                steps = [(b, c) for b in range(NB) for c in range(128)]
                PF = NU - 1

                def issue_uv(i):
                    if i >= len(steps):
                        return
                    _, c = steps[i]
                    S.dma("sp", uvslot[i % NU][:], self.uvb[c * 128:(c + 1) * 128, :], reads=[self.r_uvb], writes=[r_uv[i % NU]])

                NL = 4
                L1 = [sb("L1_%d" % i, [128, 4, 128], BF16) for i in range(NL)]
                L2w = [sb("L2w_%d" % i, [128, 4, 128], BF16) for i in range(NL)]
                r_L1 = [S.res("L1_%d" % i) for i in range(NL)]
                r_L2w = [S.res("L2w_%d" % i) for i in range(NL)]
                nh = sb("nh", [128, 1], F32)
                r_nh = S.res("nh")
                S.op("pool", lambda e: e.memset(nh[:], -0.5), writes=[r_nh])
                ssqx = sb("ssqx", [128, 4], F32)
                rstx = sb("rstx", [128, 4], F32)
                r_sq = [S.res("ssqx%d" % i) for i in range(4)]
                pending = {}
                r_aslot = [S.res("aslot%d" % i) for i in range(4)]

                def at(step, fn):
                    pending.setdefault(step, []).append(fn)

                def load_idx(bb):
                    S.dma("pool", idxb[bb % 2][:], idxw_d[:, :, bb * TB:(bb + 1) * TB], reads=r_idxd[bb * 2:bb * 2 + 2], writes=[r_idxb[bb % 2]])

                def rstd_ops(col, s):
                    at(s, lambda: S.op("pool", lambda e: e.tensor_scalar(out=rstx[:, col:col + 1], in0=ssqx[:, col:col + 1], scalar1=1.0 / D, scalar2=EPS,
                                                                          op0=ALU.mult, op1=ALU.add), reads=[r_sq[col]], writes=[r_sq[col]]))
                    at(s + 2, lambda: S.op("pool", lambda e: e.tensor_tensor(out=rstx[:, col:col + 1], in0=rstx[:, col:col + 1], in1=nh[:], op=ALU.pow),
                                           reads=[r_sq[col], r_nh], writes=[r_sq[col]]))

                def sched_load_norm(bb, tt, s):
                    hT, rhT = h2T[bb % 2], r_h2T[bb % 2]
                    r0 = bb * TB + tt * 128
                    col = tt
                    at(s, lambda: S.dma("pool", xs[:], self.x2s[r0:r0 + 128, :], writes=[r_xs]))
                    at(s + 3, lambda: S.op("act", lambda e: e.activation(out=junk[:], in_=xs[:], func=AF.Square, accum_out=ssqx[:, col:col + 1]),
                                           reads=[r_xs], writes=[r_junk, r_sq[col]]))
                    rstd_ops(col, s + 5)
                    at(s + 9, lambda: S.op("dve", lambda e: e.scalar_tensor_tensor(out=h2[:], in0=xs[:], scalar=rstx[:, col:col + 1], in1=g2bc[:],
                                                                                    op0=ALU.mult, op1=ALU.mult), reads=[r_xs, r_sq[col], r_c], writes=[r_h2]))

                    def tr():
                        bk, rb = self.banks[7], self.rbank[7]
                        bkb = bk[:].bitcast(BF16)
                        for k in range(8):
                            S.op("pe", lambda e, k=k: e.transpose(out=bkb[:, k * 128:(k + 1) * 128], in_=h2[:, k * 128:(k + 1) * 128], identity=self.ident_bf[:]),
                                 reads=[r_h2, self.r_const], writes=[rb])
                    at(s + 11, tr)

                    def ev():
                        bk, rb = self.banks[7], self.rbank[7]
                        bkb = bk[:].bitcast(BF16)
                        S.op("act", lambda e: e.activation(out=hT[:, :, tt * 128:(tt + 1) * 128], in_=bkb.rearrange("p (k t) -> p k t", k=8), func=AF.Copy),
                             reads=[rb], writes=[rhT])
                    at(s + 13, ev)

                def sched_g_item(bb, j, s):
                    li = j % NL
                    ib, rib = idxb[bb % 2], r_idxb[bb % 2]

                    def dv():
                        for q in range(4):
                            t = 4 * j + q
                            S.op("dve", lambda e: e.tensor_scalar(out=L1[li][:, q, :], in0=iota_bf[:], scalar1=ib[:, 0, t:t + 1], scalar2=None, op0=ALU.is_equal),
                                 reads=[r_io, rib], writes=[r_L1[li]])
                            S.op("dve", lambda e: e.tensor_scalar(out=L2w[li][:, q, :], in0=iota_bf[:], scalar1=ib[:, 1, t:t + 1], scalar2=ib[:, 2, t:t + 1],
                                                                  op0=ALU.is_equal, op1=ALU.mult), reads=[r_io, rib], writes=[r_L2w[li]])

                    def pe():
                        bk, rb = self.banks[7], self.rbank[7]
                        for q in range(4):
                            S.op("pe", lambda e: e.matmul(bk[:, q * 128:(q + 1) * 128], lhsT=L1[li][:, q, :], rhs=L2w[li][:, q, :], start=True, stop=True),
                                 reads=[r_L1[li], r_L2w[li]], writes=[rb])

                    def ac():
                        bk, rb = self.banks[7], self.rbank[7]
                        S.op("act", lambda e: e.activation(out=Gs[bb % 2][:, 4 * j:4 * j + 4, :], in_=bk[:].rearrange("p (q n) -> p q n", q=4), func=AF.Copy),
                             reads=[rb], writes=[r_Gs[bb % 2][j]])
                    at(s, dv)
                    at(s + 3, pe)
                    at(s + 4, ac)

                def sched_next_block(bb, s0):
                    at(s0, lambda: load_idx(bb))
                    sched_load_norm(bb, 0, s0)
                    sched_load_norm(bb, 1, s0 + 12)
                    for j in range(TB // 4):
                        sched_g_item(bb, j, s0 + 28 + (j * 3) // 2)

                def emit_A(b, c):
                    i = b * 128 + c
                    us, rus = uvslot[i % NU][:, 0:1024], r_uv[i % NU]
                    sl = c % 3
                    bk, rb = self.banks[4 + sl], self.rbank[4 + sl]
                    co = 0
                    for k in range(8):
                        S.op("pe", lambda e, k=k: e.matmul(bk[:, co:co + TB], lhsT=us[:, k * 128:(k + 1) * 128], rhs=h2T[b % 2][:, k, :], start=(k == 0), stop=(k == 7)),
                             reads=[rus, r_h2T[b % 2]], writes=[rb])

                def emit_rest(b, c):
                    i = b * 128 + c
                    vs, rvs = uvslot[i % NU][:, 1024:2048], r_uv[i % NU]
                    sl = c % 3
                    bk, rb = self.banks[4 + sl], self.rbank[4 + sl]
                    co = 0
                    gg, rgg = ga[i % 2], r_ga[i % 2]
                    S.op("act", lambda e: e.activation(out=gg[:], in_=bk[:, co:co + TB], func=AF.Gelu), reads=[rb], writes=[rgg])
                    gt, rgt = gaT[i % 2], r_gaT[i % 2]
                    S.op("dve", lambda e: e.tensor_tensor(out=gt[:], in0=gg[:], in1=Gs[b % 2][:, :, c], op=ALU.mult), reads=[rgg] + r_Gs[b % 2], writes=[rgt])
                    for tt in range(2):
                        for half in range(2):
                            ai = tt * 2 + half
                            S.op("pe", lambda e: e.matmul(self.banks[ai][:, :], lhsT=gt[:, tt * 128:(tt + 1) * 128], rhs=vs[:, half * 512:(half + 1) * 512],
                                                          start=(c == 0), stop=(c == 127)), reads=[rgt, rvs], writes=[self.rbank[ai]])
                    issue_uv(i + NU)

                def final_evac(b):
                    for tt in range(2):
                        for half in range(2):
                            ai = tt * 2 + half
                            S.op("dve", lambda e: e.tensor_tensor(out=x2b[:, tt, half * 512:(half + 1) * 512], in0=self.banks[ai][:, :],
                                                                  in1=x2b[:, tt, half * 512:(half + 1) * 512], op=ALU.add),
                                 reads=[self.rbank[ai], r_x2b], writes=[r_x2b])

                def sched_final_norm(b, s):
                    for tt in range(2):
                        col = 2 + tt
                        s1 = s + 2 + tt * 3
                        at(s1, lambda tt=tt, col=col: S.op("act", lambda e: e.activation(out=junk[:], in_=x2b[:, tt, :], func=AF.Square, accum_out=ssqx[:, col:col + 1]),
                                                           reads=[r_x2b], writes=[r_junk, r_sq[col]]))
                        rstd_ops(col, s1 + 2)
                        at(s1 + 7, lambda tt=tt, col=col: S.op("dve", lambda e: e.scalar_tensor_tensor(out=x2b[:, tt, :], in0=x2b[:, tt, :], scalar=rstx[:, col:col + 1], in1=gfbc[:],
                                                                                                    op0=ALU.mult, op1=ALU.mult), reads=[r_x2b, r_sq[col], r_c], writes=[r_x2b]))
                    at(s + 15, lambda: S.dma("pool", self.y[b * TB:(b + 1) * TB, :].rearrange("(tt p) d -> p tt d", p=128), x2b[:], reads=[r_x2b], writes=[r_yd], semres=r_x2b))

                def run_pending(step):
                    for fn in pending.pop(step, []):
                        fn()

                for i in range(NU):
                    issue_uv(i)
                sched_next_block(0, -200)
                for s in sorted(pending):
                    run_pending(s)
                for b in range(NB):
                    s0 = b * 128
                    if b + 1 < NB:
                        sched_next_block(b + 1, s0)
                    at(s0 + 40, lambda b=b: S.dma("pool", x2b[:], self.x2s[b * TB:(b + 1) * TB, :].rearrange("(tt p) d -> p tt d", p=128), writes=[r_x2b]))
                    emit_A(b, 0)
                    emit_A(b, 1)
                    for c in range(128):
                        if c + 2 < 128:
                            emit_A(b, c + 2)
                        emit_rest(b, c)
                        run_pending(s0 + c)
                    final_evac(b)
                    sched_final_norm(b, s0 + 128)
                for s in sorted(pending):
                    run_pending(s)
            S.barrier()


def host_layout(inputs):
    f = lambda a: np.ascontiguousarray(np.asarray(a, dtype=np.float32))
    w = {}
    w["w_in"] = f(inputs["w_in"][0])
    w["pa"] = f(inputs["w_branch_hg"][0])
    w["pb"] = f(inputs["w_branch_conv"][0])
    w["wo"] = f(inputs["w_out"][0])
    w["wq"] = f(inputs["peer_w_query"][0])
    k1 = np.asarray(inputs["peer_keys1"][0])
    k2 = np.asarray(inputs["peer_keys2"][0])
    ks = np.stack([k1, k2], 0)
    w["keysT"] = f(ks.transpose(3, 0, 1, 2).reshape(128, 16 * 128))
    U = np.asarray(inputs["peer_u"][0])
    w["uT"] = f(U.reshape(128, 128, 8, 128).transpose(1, 3, 2, 0).reshape(128 * 128, 1024))
    V = np.asarray(inputs["peer_v"][0])
    w["vC"] = f(V.reshape(128, 128, 1024).transpose(1, 0, 2).reshape(128 * 128, 1024))
    w["g1bc"] = f(np.broadcast_to(np.asarray(inputs["norm_mix_g"][0])[None, :], (128, D)))
    w["g2bc"] = f(np.broadcast_to(np.asarray(inputs["norm_ffn_g"][0])[None, :], (128, D)))
    w["gfbc"] = f(np.broadcast_to(np.asarray(inputs["norm_final_g"])[None, :], (128, D)))
    w["gobc"] = f(np.broadcast_to(np.asarray(inputs["hg_out_norm_g"][0])[None, :], (64, 512)))
    lg = np.asarray(inputs["hg_lb_logits"])
    w["lbl"] = f(lg.reshape(2, 8, 64).transpose(2, 0, 1).reshape(64, 16))
    cw = np.asarray(inputs["conv_w"][0])
    w["convw"] = f(cw.reshape(3, 4, 128).transpose(2, 1, 0).reshape(128, 12))
    return w


_PROG_CACHE = {}


def run(inputs, T, ncores, dbg=None):
    key = (T, dbg)
    if key not in _PROG_CACHE:
        _PROG_CACHE[key] = Prog(T, dbg)
    prog = _PROG_CACHE[key]
    w = host_layout(inputs)
    x = np.asarray(inputs["x"], dtype=np.float32)
    in_maps = []
    for c in range(ncores):
        m = dict(w)
        m["x"] = np.ascontiguousarray(x[c, :T, :])
        in_maps.append(m)
    res = run_bass_kernel_spmd(prog.nc, in_maps, core_ids=list(range(ncores)))
    return res


def kernel(**inputs):
    res = run(inputs, 4096, 8)
    out = np.stack([np.asarray(r["y"], dtype=np.float32) for r in res.results], 0)
    return out
```
